# Optimizing a Trainium2 kernel written in Bass

```python
import math
import jax
import jax.numpy as jnp
from jax import lax
import numpy as np

D_MODEL = 1024
BATCH = 8
SEQ = 2048
DEPTH = 4

DN_HEADS = 4
DN_DK = 128
DN_DV = 128
DN_CONV = 4
DN_CHUNK = 64
DN_QK = DN_HEADS * DN_DK
DN_WIDTH = DN_HEADS * DN_DV
SA_HEADS = 8
SA_DK = 64
SA_DV = 64
SA_Q_RANK = 256
SA_KV_RANK = 128
SA_WIDTH = SA_HEADS * SA_DV
IDX_HEADS = 8
IDX_DIM = 64
TOPK_MAX = 256
Q_BLOCK = 128
MIX_WIDTH = DN_WIDTH + SA_WIDTH
IN_SIZES = (DN_QK, DN_QK, DN_WIDTH, DN_WIDTH, DN_HEADS, DN_HEADS, SA_Q_RANK, SA_KV_RANK, IDX_DIM, IDX_HEADS)
IN_WIDTH = sum(IN_SIZES)
N_EXPERTS = 32
TOP_K = 4
D_FF = 1024
SWIGLU_LIMIT = 7.0
SWIGLU_ALPHA = 1.702
EPS = 1e-6
DEEPNORM_ALPHA = (2 * DEPTH) ** 0.25
DEEPNORM_BETA = (8 * DEPTH) ** -0.25

kernel_name = 'hybrid_deltanet_dsa_moe_deepnorm'


def _rmsnorm(x, g):
    xf = x.astype(jnp.float32)
    y = xf * lax.rsqrt(jnp.mean(xf * xf, axis=-1, keepdims=True) + EPS)
    return (y * g.astype(jnp.float32)).astype(x.dtype)


def _layernorm(x, g, b):
    xf = x.astype(jnp.float32)
    mu = jnp.mean(xf, axis=-1, keepdims=True)
    var = jnp.mean(jnp.square(xf - mu), axis=-1, keepdims=True)
    y = (xf - mu) * lax.rsqrt(var + EPS)
    return (y * g.astype(jnp.float32) + b.astype(jnp.float32)).astype(x.dtype)


def _l2norm(x):
    xf = x.astype(jnp.float32)
    return xf * lax.rsqrt(jnp.sum(xf * xf, axis=-1, keepdims=True) + EPS)


def _causal_dwconv(x, w):
    c = x.shape[-1]
    kw = w.shape[0]
    return lax.conv_general_dilated(
        x, w[:, None, :].astype(x.dtype), window_strides=(1,), padding=[(kw - 1, 0)],
        dimension_numbers=('NWC', 'WIO', 'NWC'), feature_group_count=c)


def _gated_delta_rule(q, k, v, g, beta):
    f32 = jnp.float32
    b, t, h, dk = q.shape
    dv = v.shape[-1]
    c = DN_CHUNK
    n = t // c

    def chunks(a):
        a = a.astype(f32).reshape((b, n, c, h) + a.shape[3:])
        return jnp.moveaxis(a, 3, 1)

    q = chunks(q) * (dk ** -0.5)
    k = chunks(k)
    v = chunks(v)
    g = jnp.cumsum(chunks(g), axis=-1)
    beta = chunks(beta)
    k_beta = k * beta[..., None]
    v_beta = v * beta[..., None]
    tril = jnp.tril(jnp.ones((c, c), bool))
    strict = jnp.tril(jnp.ones((c, c), bool), -1)
    decay = jnp.exp(jnp.where(tril, g[..., :, None] - g[..., None, :], -jnp.inf))
    lower = jnp.where(strict, jnp.einsum('bhncd,bhnsd->bhncs', k_beta, k) * decay, 0.0)
    eye = jnp.eye(c, dtype=f32)
    rhs = jnp.concatenate([v_beta, k_beta * jnp.exp(g)[..., None]], axis=-1)
    sol = lax.linalg.triangular_solve(eye + lower, rhs, left_side=True, lower=True, unit_diagonal=True)
    u, w = sol[..., :dv], sol[..., dv:]
    attn_intra = jnp.einsum('bhncd,bhnsd->bhncs', q, k) * decay
    g_last = g[..., -1]
    k_dec = k * jnp.exp(g_last[..., None] - g)[..., None]
    q_dec = q * jnp.exp(g)[..., None]

    def step(state, xs):
        qd, kd, uc, wc, a, gl = xs
        v_new = uc - jnp.einsum('bhck,bhkv->bhcv', wc, state)
        o = jnp.einsum('bhck,bhkv->bhcv', qd, state) + jnp.einsum('bhcs,bhsv->bhcv', a, v_new)
        state = state * jnp.exp(gl)[..., None, None] + jnp.einsum('bhck,bhcv->bhkv', kd, v_new)
        return state, o

    xs = tuple(jnp.moveaxis(a, 2, 0) for a in (q_dec, k_dec, u, w, attn_intra, g_last))
    s0 = jnp.zeros((b, h, dk, dv), f32)
    _, o = lax.scan(step, s0, xs)
    return jnp.transpose(o, (1, 0, 3, 2, 4)).reshape(b, t, h, dv)


def _gated_deltanet(dq, dk, dv, dz, db, da, conv_w, a_log, dt_bias, norm_w):
    b, t, _ = dq.shape
    qkv = jax.nn.silu(_causal_dwconv(jnp.concatenate([dq, dk, dv], axis=-1), conv_w))
    q, k, v = jnp.split(qkv, [DN_QK, 2 * DN_QK], axis=-1)
    q = _l2norm(q.reshape(b, t, DN_HEADS, DN_DK))
    k = _l2norm(k.reshape(b, t, DN_HEADS, DN_DK))
    v = v.reshape(b, t, DN_HEADS, DN_DV)
    beta = jax.nn.sigmoid(db.astype(jnp.float32))
    g = -jnp.exp(a_log.astype(jnp.float32)) * jax.nn.softplus(da.astype(jnp.float32) + dt_bias.astype(jnp.float32))
    o = _gated_delta_rule(q, k, v, g, beta)
    z = dz.reshape(b, t, DN_HEADS, DN_DV).astype(jnp.float32)
    o = _rmsnorm(o, norm_w) * jax.nn.silu(z)
    return o.reshape(b, t, DN_WIDTH).astype(dq.dtype)


def _dsa(c_q, c_kv, idx_k, idx_w, q_norm, w_uq, kv_norm, w_uk, w_uv, w_qidx, idxk_g, idxk_b):
    b, t, _ = c_q.shape
    k_top = min(TOPK_MAX, t // 4)
    nb = t // Q_BLOCK
    cq = _rmsnorm(c_q, q_norm)
    q = (cq @ w_uq).reshape(b, t, SA_HEADS, SA_DK)
    q_lat = jnp.einsum('bthd,hdr->bthr', q, w_uk) * (SA_DK ** -0.5)
    q_idx = (cq @ w_qidx).reshape(b, t, IDX_HEADS, IDX_DIM)
    ckv = _rmsnorm(c_kv, kv_norm)
    k_idx = _layernorm(idx_k, idxk_g, idxk_b)
    w_head = idx_w * ((IDX_HEADS ** -0.5) * (IDX_DIM ** -0.5))
    key_pos = jnp.arange(t)

    def blockify(a):
        return jnp.moveaxis(a.reshape((b, nb, Q_BLOCK) + a.shape[2:]), 1, 0)

    def one_block(args):
        qi, qlat_b, qidx_b, w_b = args
        t_pos = qi * Q_BLOCK + jnp.arange(Q_BLOCK)
        causal = key_pos[None, :] <= t_pos[:, None]
        logits = jax.nn.relu(jnp.einsum('bqhd,bsd->bqhs', qidx_b, k_idx))
        score = jnp.einsum('bqh,bqhs->bqs', w_b, logits).astype(jnp.float32)
        score = jnp.where(causal[None], score, -jnp.inf)
        _, sel = lax.top_k(score, k_top)
        valid = sel <= t_pos[None, :, None]
        c_sel = jax.vmap(lambda cc, ii: cc[ii])(ckv, sel)
        s = jnp.einsum('bqhr,bqkr->bqhk', qlat_b, c_sel).astype(jnp.float32)
        s = jnp.where(valid[:, :, None, :], s, -jnp.inf)
        p = jax.nn.softmax(s, axis=-1).astype(c_sel.dtype)
        return jnp.einsum('bqhk,bqkr->bqhr', p, c_sel)

    o_lat = lax.map(one_block, (jnp.arange(nb), blockify(q_lat), blockify(q_idx), blockify(w_head)))
    o_lat = jnp.moveaxis(o_lat, 0, 1).reshape(b, t, SA_HEADS, SA_KV_RANK)
    o = jnp.einsum('bthr,hrv->bthv', o_lat, w_uv)
    return o.reshape(b, t, SA_WIDTH)


def _moe(x, router_w, router_b, w_gu, b_gu, w_dn, b_dn):
    b, t, d = x.shape
    xt = x.reshape(b * t, d)
    logits = (xt @ router_w + router_b).astype(jnp.float32)
    top_val, top_idx = lax.top_k(logits, TOP_K)
    gates = jax.nn.softmax(top_val, axis=-1)
    combine = jnp.sum(jax.nn.one_hot(top_idx, N_EXPERTS, dtype=jnp.float32) * gates[..., None], axis=1)

    def expert(acc, xs):
        wgu, bgu, wd, bd, cw = xs
        gate, up = jnp.split(xt @ wgu + bgu, 2, axis=-1)
        gate = jnp.minimum(gate, SWIGLU_LIMIT)
        up = jnp.clip(up, -SWIGLU_LIMIT, SWIGLU_LIMIT)
        hdn = (up + 1.0) * (gate * jax.nn.sigmoid(SWIGLU_ALPHA * gate))
        y = hdn @ wd + bd
        return acc + cw[:, None].astype(y.dtype) * y, None

    acc, _ = lax.scan(expert, jnp.zeros_like(xt), (w_gu, b_gu, w_dn, b_dn, combine.T))
    return acc.reshape(b, t, d)


def setup_inputs(seed: int = 0) -> dict:
    key = jax.random.key(seed)
    ks = jax.random.split(key, 32)
    f32 = jnp.float32
    L = DEPTH
    conv_ch = 2 * DN_QK + DN_WIDTH

    def nrm(k, shape, scale):
        return jax.random.normal(k, shape, f32) * scale

    dt = jnp.exp(jax.random.uniform(ks[4], (L, DN_HEADS), f32, math.log(1e-3), math.log(1e-1)))
    return {
        'x': nrm(ks[0], (BATCH, SEQ, D_MODEL), 1.0),
        'w_in': nrm(ks[1], (L, D_MODEL, IN_WIDTH), D_MODEL ** -0.5),
        'dn_conv': nrm(ks[2], (L, DN_CONV, conv_ch), DN_CONV ** -0.5),
        'dn_a_log': jnp.log(jax.random.uniform(ks[3], (L, DN_HEADS), f32, 1.0, 16.0)),
        'dn_dt_bias': dt + jnp.log(-jnp.expm1(-dt)),
        'dn_norm': 1.0 + nrm(ks[5], (L, DN_DV), 0.02),
        'sa_q_norm': 1.0 + nrm(ks[6], (L, SA_Q_RANK), 0.02),
        'sa_w_uq': nrm(ks[7], (L, SA_Q_RANK, SA_HEADS * SA_DK), SA_Q_RANK ** -0.5),
        'sa_kv_norm': 1.0 + nrm(ks[8], (L, SA_KV_RANK), 0.02),
        'sa_w_uk': nrm(ks[9], (L, SA_HEADS, SA_DK, SA_KV_RANK), SA_KV_RANK ** -0.5),
        'sa_w_uv': nrm(ks[10], (L, SA_HEADS, SA_KV_RANK, SA_DV), SA_KV_RANK ** -0.5),
        'idx_w_q': nrm(ks[11], (L, SA_Q_RANK, IDX_HEADS * IDX_DIM), SA_Q_RANK ** -0.5),
        'idx_k_norm_g': 1.0 + nrm(ks[12], (L, IDX_DIM), 0.02),
        'idx_k_norm_b': nrm(ks[13], (L, IDX_DIM), 0.02),
        'w_o': nrm(ks[14], (L, MIX_WIDTH, D_MODEL), (MIX_WIDTH ** -0.5) * DEEPNORM_BETA),
        'ln1_g': 1.0 + nrm(ks[15], (L, D_MODEL), 0.02),
        'ln1_b': nrm(ks[16], (L, D_MODEL), 0.02),
        'router_w': nrm(ks[17], (L, D_MODEL, N_EXPERTS), D_MODEL ** -0.5),
        'router_b': nrm(ks[18], (L, N_EXPERTS), 0.01),
        'w_gate_up': nrm(ks[19], (L, N_EXPERTS, D_MODEL, 2 * D_FF), D_MODEL ** -0.5),
        'b_gate_up': nrm(ks[20], (L, N_EXPERTS, 2 * D_FF), 0.02),
        'w_down': nrm(ks[21], (L, N_EXPERTS, D_FF, D_MODEL), (D_FF ** -0.5) * DEEPNORM_BETA),
        'b_down': nrm(ks[22], (L, N_EXPERTS, D_MODEL), 0.02),
        'ln2_g': 1.0 + nrm(ks[23], (L, D_MODEL), 0.02),
        'ln2_b': nrm(ks[24], (L, D_MODEL), 0.02),
    }


def reference(x, w_in, dn_conv, dn_a_log, dn_dt_bias, dn_norm, sa_q_norm, sa_w_uq, sa_kv_norm, sa_w_uk, sa_w_uv, idx_w_q, idx_k_norm_g, idx_k_norm_b, w_o, ln1_g, ln1_b, router_w, router_b, w_gate_up, b_gate_up, w_down, b_down, ln2_g, ln2_b):
    splits = [int(s) for s in np.cumsum(IN_SIZES)[:-1]]
    for l in range(DEPTH):
        proj = x @ w_in[l]
        dq, dk, dv, dz, db, da, cq, ckv, ik, iw = jnp.split(proj, splits, axis=-1)
        y_dn = _gated_deltanet(dq, dk, dv, dz, db, da, dn_conv[l], dn_a_log[l], dn_dt_bias[l], dn_norm[l])
        y_sa = _dsa(cq, ckv, ik, iw, sa_q_norm[l], sa_w_uq[l], sa_kv_norm[l], sa_w_uk[l], sa_w_uv[l],
                    idx_w_q[l], idx_k_norm_g[l], idx_k_norm_b[l])
        mix = jnp.concatenate([y_dn.astype(x.dtype), y_sa.astype(x.dtype)], axis=-1) @ w_o[l]
        x = _layernorm(DEEPNORM_ALPHA * x + mix, ln1_g[l], ln1_b[l])
        ffn = _moe(x, router_w[l], router_b[l], w_gate_up[l], b_gate_up[l], w_down[l], b_down[l])
        x = _layernorm(DEEPNORM_ALPHA * x + ffn, ln2_g[l], ln2_b[l])
    return x
```

```python
import numpy as np
import concourse.bass as bass
import concourse.mybir as mybir
from concourse.bass_utils import run_bass_kernel_spmd
from contextlib import ExitStack

F32 = mybir.dt.float32
BF16 = mybir.dt.bfloat16
AF = mybir.ActivationFunctionType
ALU = mybir.AluOpType
AX = mybir.AxisListType

T = 2048
D = 1024
NT = 16
NL = 4
NE = 32
ALPHA = float(8 ** 0.25)
EPS = 1e-6
NBIS = 20
DBG = dict(tiles=NT, stage=99)
TOPK = 256


class Sched:
    ENG = ("pe", "act", "dve", "pool", "sp")

    def __init__(self, nc, es, plan=None):
        self.nc = nc
        self.es = es
        self.plan = plan
        self.dry = plan is None
        self.needed = {n: set() for n in self.ENG}
        self.E = {}
        engs = dict(pe=nc.tensor, act=nc.scalar, dve=nc.vector, pool=nc.gpsimd, sp=nc.sync)
        for name in self.ENG:
            sem = None if self.dry else es.enter_context(nc.semaphore(name + "_sem"))
            self.E[name] = dict(eng=engs[name], sem=sem, count=0, seen={}, name=name)
        if not self.dry:
            self.rank = {}
            for n in self.ENG:
                vals = sorted(plan[n])
                self.rank[n] = {v: i + 1 for i, v in enumerate(vals)}
        self.res = {}
        self.dsem = {}
        self.nwaits = 0
        self.ninst = 0

    def _r(self, key):
        r = self.res.get(key)
        if r is None:
            r = self.res[key] = dict(w=None, r={})
        return r

    def _dma_sem(self, key):
        d = self.dsem.get(key)
        if d is None:
            sem = None if self.dry else self.es.enter_context(self.nc.semaphore("d_%d" % len(self.dsem)))
            d = self.dsem[key] = [sem, 0, "dma:" + str(key)]
        return d

    def _collect(self, ename, reads, writes):
        need = {}

        def add(dep, kind):
            if dep is None:
                return
            sid, val, src = dep
            if src == ename:
                if ename in ("pe", "sp") or kind == "war":
                    return
            cur = need.get(sid)
            if cur is None or cur < val:
                need[sid] = val

        for k in reads:
            add(self._r(k)["w"], "raw")
        for k in writes:
            r = self._r(k)
            add(r["w"], "waw")
            for dep in r["r"].values():
                add(dep, "war")
        return need

    def _emit_waits(self, ename, need):
        E = self.E[ename]
        for sid, val in need.items():
            if E["seen"].get(sid, 0) >= val:
                continue
            E["seen"][sid] = val
            self.nwaits += 1
            if sid in self.E:
                if self.dry:
                    self.needed[sid].add(val)
                else:
                    E["eng"].wait_ge(self.E[sid]["sem"], self.rank[sid][val])
            else:
                if not self.dry:
                    E["eng"].wait_ge(self._dsem_by_id[sid], val)

    def op(self, ename, fn, reads=(), writes=()):
        E = self.E[ename]
        self._emit_waits(ename, self._collect(ename, reads, writes))
        E["count"] += 1
        ins = None
        if not self.dry:
            ins = fn(E["eng"])
            if E["count"] in self.rank[ename]:
                ins.then_inc(E["sem"], 1)
        dep = (ename, E["count"], ename)
        for k in reads:
            self._r(k)["r"][dep[0]] = dep
        for k in writes:
            r = self._r(k)
            r["w"] = dep
            r["r"] = {}
        self.ninst += 1
        return ins

    def dma(self, qname, out, in_, reads=(), writes=(), semkey=None, **kw):
        E = self.E[qname]
        self._emit_waits(qname, self._collect("dma", reads, writes))
        d = self._dma_sem(semkey if semkey is not None else writes[0])
        d[1] += 16
        if not self.dry:
            if not hasattr(self, "_dsem_by_id"):
                self._dsem_by_id = {}
            self._dsem_by_id[d[2]] = d[0]
            ins = E["eng"].dma_start(out=out, in_=in_, **kw)
            ins.then_inc(d[0], 16)
        dep = (d[2], d[1], "dma")
        for k in reads:
            self._r(k)["r"][dep[0]] = dep
        for k in writes:
            r = self._r(k)
            r["w"] = dep
            r["r"] = {}
        self.ninst += 1

    def barrier(self):
        need = {}
        for n, E in self.E.items():
            if E["count"]:
                need[n] = E["count"]
        for d in self.dsem.values():
            if d[1]:
                need[d[2]] = d[1]
        for name in self.E:
            self._emit_waits(name, need)
        self.res = {}

    def mm(self, out, lhsT, rhs, start, stop, r, w):
        return self.op("pe", lambda e: e.matmul(out, lhsT, rhs, start=start, stop=stop), r, w)

    def tr(self, out, in_, ident, r, w):
        return self.op("pe", lambda e: e.transpose(out, in_, ident), list(r) + ["const"], w)

    def act(self, out, in_, func, r, w, **kw):
        return self.op("act", lambda e: e.activation(out, in_, func, **kw), r, w)

    def ts(self, eng, out, in0, s1, s2, op0, op1, r, w, **kw):
        if s2 is None:
            return self.op(eng, lambda e: e.tensor_scalar(out, in0, s1, None, op0, **kw), r, w)
        return self.op(eng, lambda e: e.tensor_scalar(out, in0, s1, s2, op0, op1, **kw), r, w)

    def tt(self, eng, out, in0, in1, op, r, w):
        return self.op(eng, lambda e: e.tensor_tensor(out, in0, in1, op), r, w)

    def stt(self, eng, out, in0, sc, in1, op0, op1, r, w):
        return self.op(eng, lambda e: e.scalar_tensor_tensor(out, in0, sc, in1, op0, op1), r, w)

    def cp(self, eng, out, in_, r, w):
        if eng == "act":
            return self.op("act", lambda e: e.copy(out, in_), r, w)
        return self.op(eng, lambda e: e.tensor_copy(out, in_), r, w)


class Ctx:
    pass


_uid = [0]


def uid(n):
    _uid[0] += 1
    return "%s_%d" % (n, _uid[0])


def rstd_from_ss(S, A, ss, n, tag, rk):
    S.act(ss, ss, AF.Sqrt, [rk], [rk], scale=1.0 / n, bias=A.eps[:, 0:1])
    S.op("dve", lambda e: e.reciprocal(ss, ss), [rk], [rk])


def setup_consts(C, es):
    nc, S = C.nc, C.S
    A = C
    A.ident = es.enter_context(nc.sbuf_tensor("ident", [128, 128], F32))
    A.identb = es.enter_context(nc.sbuf_tensor("identb", [128, 128], BF16))
    A.ones = es.enter_context(nc.sbuf_tensor("ones", [128, 128], F32))
    A.onesb = es.enter_context(nc.sbuf_tensor("onesb", [128, 128], BF16))
    A.tril = es.enter_context(nc.sbuf_tensor("tril", [128, 128], F32))
    A.trils = es.enter_context(nc.sbuf_tensor("trils", [128, 128], F32))
    A.triu = es.enter_context(nc.sbuf_tensor("triu", [128, 128], F32))
    A.cmask = es.enter_context(nc.sbuf_tensor("cmask", [128, 128], F32))
    A.eps = es.enter_context(nc.sbuf_tensor("epsc", [128, 1], F32))
    g = nc.gpsimd
    k = ["const"]
    S.op("pool", lambda e: e.memset(A.ident[:], 0.0), [], k)
    S.op("pool", lambda e: e.affine_select(out=A.ident[:], in_=A.ident[:], pattern=[[-1, 128]],
                                           compare_op=ALU.not_equal, fill=1.0, base=0, channel_multiplier=1), k, k)
    S.op("pool", lambda e: e.memset(A.ones[:], 1.0), [], k)
    S.op("pool", lambda e: e.memset(A.eps[:], EPS), [], k)
    S.op("pool", lambda e: e.affine_select(out=A.tril[:], in_=A.ones[:], pattern=[[-1, 128]],
                                           compare_op=ALU.is_ge, fill=0.0, base=0, channel_multiplier=1), k, k)
    S.op("pool", lambda e: e.affine_select(out=A.trils[:], in_=A.ones[:], pattern=[[-1, 128]],
                                           compare_op=ALU.is_gt, fill=0.0, base=0, channel_multiplier=1), k, k)
    S.op("pool", lambda e: e.affine_select(out=A.triu[:], in_=A.ones[:], pattern=[[1, 128]],
                                           compare_op=ALU.is_ge, fill=0.0, base=0, channel_multiplier=-1), k, k)
    S.ts("pool", A.cmask[:], A.tril[:], -1.0, 1e30, ALU.add, ALU.mult, k, k)
    S.cp("pool", A.identb[:], A.ident[:], k, k)
    S.cp("pool", A.onesb[:], A.ones[:], k, k)


def phase_dsa(C, l):
    nc, S = C.nc, C.S
    xT, olT = C.xT, C.olT
    with ExitStack() as es:
        A = lambda n, sh, dt: es.enter_context(nc.sbuf_tensor(uid(n), sh, dt))
        P = lambda n, sh, dt: es.enter_context(nc.psum_tensor(uid(n), sh, dt))
        w_dsa = A("w_dsa", [128, 8, 456], BF16)
        wqidx = A("wqidx", [128, 2, 512], F32)
        wuqT = A("wuqT", [64, 8, 256], F32)
        wuk = A("wuk", [64, 8, 128], F32)
        qn = A("qn", [128, 256], F32)
        kvn = A("kvn", [128, 128], F32)
        ig = A("ig", [128, 64], F32)
        ib = A("ib", [128, 64], F32)
        Wql = A("Wql", [128, 2, 8, 128], F32)
        kT2 = A("kT2", [128, T], F32)
        ckvT = A("ckvT", [128, T], BF16)
        ckvt = A("ckvt", [128, NT, 128], BF16)
        pj = [A("pj", [128, 456], F32) for _ in range(2)]
        cqn = [A("cqn", [128, 256], F32) for _ in range(2)]
        kdup = [A("kdup", [128, 128], F32) for _ in range(2)]
        st = [A("st", [128, 16], F32) for _ in range(2)]
        wsm = [A("wsm", [128, 16], F32) for _ in range(2)]
        cqnT = [A("cqnT", [128, 2, 128], F32) for _ in range(2)]
        qiT = [A("qiT", [128, 4, 128], F32) for _ in range(2)]
        qlT = [A("qlT", [128, 8, 128], BF16) for _ in range(2)]
        score = [A("score", [128, T], F32)] * 2
        Rt = [A("Rt", [128, 512], F32) for _ in range(2)]
        junk = A("junk", [128, T], BF16)
        junkf = A("junkf", [128, 256], F32)
        Mk = [A("Mk", [128, T], BF16)] * 2
        MT = [A("MT", [128, NT, 128], BF16) for _ in range(2)]
        pT = [A("pT", [128, 512], BF16) for _ in range(3)]
        bs = [A("bs", [128, 8], F32) for _ in range(2)]
        rinv = A("rinv", [128, 512], F32)
        pa = P("pa", [128, 512], F32)
        pb = P("pb", [128, 512], F32)
        pX = [P("pX", [128, 512], F32) for _ in range(2)]
        pMT = P("pMT", [128, 1024], BF16)
        pO = P("pO", [128, 512], F32)
        pR = P("pR", [128, 512], F32)

        S.dma("pool", w_dsa[:], C.w_in[l].rearrange("(c p) n -> p c n", p=128)[:, :, 2056:2512], [], ["w_dsa"])
        S.dma("sp", wqidx[:], C.wqidx[l].rearrange("(c p) n -> p c n", p=128), [], ["wqidx"])
        S.dma("sp", wuqT[:], C.wuqT[l].rearrange("p (h c) -> p h c", h=8), [], ["wuqT"])
        S.dma("sp", wuk[:], C.wuk[l].rearrange("p (h r) -> p h r", h=8), [], ["wuk"])
        S.dma("sp", qn[:], C.qn[l], [], ["qn"])
        S.dma("sp", kvn[:], C.kvn[l], [], ["kvn"])
        S.dma("sp", ig[:], C.ig[l], [], ["ig"])
        S.dma("sp", ib[:], C.ib[l], [], ["ib"])
        for c in range(2):
            for hg in range(2):
                pk = "pa" if hg == 0 else "pb"
                pp = pa if hg == 0 else pb
                for h4 in range(4):
                    h = hg * 4 + h4
                    S.mm(pp[:, h4 * 128:(h4 + 1) * 128], wuqT[:, h, c * 128:(c + 1) * 128], wuk[:, h, :],
                         True, True, ["wuqT", "wuk"], [pk])
                S.act(Wql[:, c, hg * 4:(hg + 1) * 4, :], pp[:].rearrange("p (h r) -> p h r", h=4), AF.Copy,
                      [pk], ["Wql"], scale=0.125)

        for i in range(DBG["tiles"]):
            b = i % 2
            nk = 128 * (i + 1)
            ts_ = slice(i * 128, (i + 1) * 128)
            for dc in range(8):
                S.mm(pa[:, 0:456], xT[:, dc, ts_], w_dsa[:, dc, :], dc == 0, dc == 7, ["xT", "w_dsa"], ["pa"])
            kpj = ("pj", b)
            S.cp("act", pj[b][:], pa[:, 0:456], ["pa"], [kpj])
            cq = pj[b][:, 0:256]
            ckv = pj[b][:, 256:384]
            ik = pj[b][:, 384:448]
            iw = pj[b][:, 448:456]
            kst = ("st", b)
            S.act(junkf[:, 0:256], cq, AF.Square, [kpj], ["junkf", kst], accum_out=st[b][:, 0:1])
            S.act(junkf[:, 0:128], ckv, AF.Square, [kpj], ["junkf", kst], accum_out=st[b][:, 1:2])
            S.act(junkf[:, 0:64], ik, AF.Identity, [kpj], ["junkf", kst], accum_out=st[b][:, 2:3], scale=1.0 / 64)
            S.act(st[b][:, 0:1], st[b][:, 0:1], AF.Sqrt, [kst], [kst], scale=1.0 / 256, bias=C.eps[:, 0:1])
            S.act(st[b][:, 1:2], st[b][:, 1:2], AF.Sqrt, [kst], [kst], scale=1.0 / 128, bias=C.eps[:, 0:1])
            S.op("dve", lambda e: e.reciprocal(st[b][:, 0:2], st[b][:, 0:2]), [kst], [kst])
            kcqn = ("cqn", b)
            S.stt("dve", cqn[b][:], cq, st[b][:, 0:1], qn[:], ALU.mult, ALU.mult, [kpj, kst, "qn"], [kcqn])
            S.stt("dve", ckvt[:, i, :], ckv, st[b][:, 1:2], kvn[:], ALU.mult, ALU.mult, [kpj, kst, "kvn"], ["ckvt"])
            kkd = ("kdup", b)
            S.ts("dve", kdup[b][:, 0:64], ik, st[b][:, 2:3], None, ALU.subtract, None, [kpj, kst], [kkd])
            S.act(junkf[:, 0:64], kdup[b][:, 0:64], AF.Square, [kkd], ["junkf", kst], accum_out=st[b][:, 3:4])
            S.act(st[b][:, 3:4], st[b][:, 3:4], AF.Sqrt, [kst], [kst], scale=1.0 / 64, bias=C.eps[:, 0:1])
            S.op("dve", lambda e: e.reciprocal(st[b][:, 3:4], st[b][:, 3:4]), [kst], [kst])
            S.stt("dve", kdup[b][:, 0:64], kdup[b][:, 0:64], st[b][:, 3:4], ig[:], ALU.mult, ALU.mult,
                  [kkd, kst, "ig"], [kkd])
            S.tt("dve", kdup[b][:, 64:128], kdup[b][:, 0:64], ib[:], ALU.add, [kkd, "ib"], [kkd])
            S.tt("dve", kdup[b][:, 0:64], kdup[b][:, 0:64], ib[:], ALU.add, [kkd, "ib"], [kkd])
            kws = ("wsm", b)
            S.act(wsm[b][:, 0:8], iw, AF.Abs, [kpj], [kws], scale=float(8 ** -0.5 * 64 ** -0.5))
            S.act(wsm[b][:, 8:16], iw, AF.Sign, [kpj], [kws])
            if DBG["stage"] < 2:
                continue
            sub = DBG.get("sub", 0)
            if sub in (0, 1, 5, 6, 7, 8):
                S.tr(pb[:, 0:128], cqn[b][:, 0:128], C.ident[:], [kcqn], ["pb"])
            if sub in (0, 1, 5, 7, 8):
                S.tr(pb[:, 128:256], cqn[b][:, 128:256], C.ident[:], [kcqn], ["pb"])
                S.tr(pb[:, 256:384], kdup[b][:], C.ident[:], [kkd], ["pb"])
            kcT = ("cqnT", b)
            if sub in (0, 1, 3, 7):
                S.cp("act", cqnT[b][:], pb[:, 0:256].rearrange("p (c q) -> p c q", c=2), ["pb"], [kcT])
            if sub in (0, 1, 4, 8):
                S.cp("act", kT2[:, ts_], pb[:, 256:384], ["pb"], ["kT2"])
            if sub in (0, 2):
                S.tr(pMT[:, 0:128], ckvt[:, i, :], C.identb[:], ["ckvt"], ["pMT"])
                S.cp("act", ckvT[:, ts_], pMT[:, 0:128], ["pMT"], ["ckvT"])
            if DBG["stage"] < 3:
                continue
            for pr in range(4):
                for c in range(2):
                    S.mm(pa[:, pr * 128:(pr + 1) * 128], wqidx[:, c, pr * 128:(pr + 1) * 128], cqnT[b][:, c, :],
                         c == 0, c == 1, ["wqidx", kcT], ["pa"])
            kqi = ("qiT", b)
            S.cp("act", qiT[b][:], pa[:].rearrange("p (h q) -> p h q", h=4), ["pa"], [kqi])
            kql = ("qlT", b)
            for hg in range(2):
                for h4 in range(4):
                    for c in range(2):
                        S.mm(pb[:, h4 * 128:(h4 + 1) * 128], Wql[:, c, hg * 4 + h4, :], cqnT[b][:, c, :],
                             c == 0, c == 1, ["Wql", kcT], ["pb"])
                S.cp("dve", qlT[b][:, hg * 4:(hg + 1) * 4, :], pb[:].rearrange("p (h q) -> p h q", h=4), ["pb"], [kql])
            if DBG["stage"] < 4:
                continue
            ksc = "score"
            nsg = (nk + 511) // 512
            cnt = 0
            for sg in range(nsg):
                w = min(512, nk - sg * 512)
                cs = slice(sg * 512, sg * 512 + w)
                for h in range(8):
                    px = pX[cnt % 2]
                    kpx = ("pX", cnt % 2)
                    kr = ("Rt", cnt % 2)
                    pr0 = (h % 2) * 64
                    S.mm(px[:, 0:w], qiT[b][pr0:pr0 + 64, h // 2, :], kT2[pr0:pr0 + 64, cs], True, True,
                         [kqi, "kT2"], [kpx])
                    S.act(Rt[cnt % 2][:, 0:w], px[:, 0:w], AF.Relu, [kpx, kws], [kr], scale=wsm[b][:, h:h + 1])
                    if h == 0:
                        S.ts("dve", score[b][:, cs], Rt[cnt % 2][:, 0:w], wsm[b][:, 8:9], None, ALU.mult, None,
                             [kr, kws], [ksc])
                    else:
                        S.stt("dve", score[b][:, cs], Rt[cnt % 2][:, 0:w], wsm[b][:, 8 + h:9 + h], score[b][:, cs],
                              ALU.mult, ALU.add, [kr, kws, ksc], [ksc])
                    cnt += 1
            kbs = ("bs", b)
            B = bs[b]
            S.op("dve", lambda e: e.tensor_reduce(B[:, 5:6], score[b][:, 0:nk], AX.X, ALU.max,
                                                  apply_absolute_value=True), [ksc], [kbs])
            S.tt("dve", score[b][:, nk - 128:nk], score[b][:, nk - 128:nk], C.cmask[:], ALU.add, [ksc, "const"], [ksc])
            S.ts("dve", B[:, 0:1], B[:, 5:6], 1.0, -1.0, ALU.add, ALU.mult, [kbs], [kbs])
            S.ts("dve", B[:, 1:2], B[:, 5:6], 1.0, 2.0, ALU.add, ALU.mult, [kbs], [kbs])

            def gen_B(i=i, b=b, nk=nk, B=B, kbs=kbs, ksc=ksc):
                for it in range(NBIS):
                    cst = float(2.0 ** -(it + 1))
                    S.stt("dve", B[:, 2:3], B[:, 1:2], cst, B[:, 0:1], ALU.mult, ALU.add, [kbs], [kbs])
                    S.ts("dve", junk[:, 0:nk], score[b][:, 0:nk], B[:, 2:3], 0.0, ALU.is_ge, ALU.add, [ksc, kbs],
                         ["junk", kbs], accum_out=B[:, 3:4])
                    S.ts("dve", B[:, 4:5], B[:, 3:4], float(TOPK), cst, ALU.is_ge, ALU.mult, [kbs], [kbs])
                    S.stt("dve", B[:, 0:1], B[:, 4:5], B[:, 1:2], B[:, 0:1], ALU.mult, ALU.add, [kbs], [kbs])
                    yield

            def gen_C(i, b):
                ts_c = slice(i * 128, (i + 1) * 128)
                kql_, kmt_ = ("qlT", b), ("MT", b)
                stp = [(hg, j) for hg in range(2) for j in range(i + 1)]

                def smm(k):
                    hg, j = stp[k]
                    qv = qlT[b][:, hg * 4:(hg + 1) * 4, :].rearrange("p h q -> p (h q)")
                    S.mm(pX[k % 2][:, :], ckvT[:, j * 128:(j + 1) * 128], qv, True, True, ["ckvT", kql_],
                         [("pX", k % 2)])

                smm(0)
                for k, (hg, j) in enumerate(stp):
                    if k + 1 < len(stp):
                        smm(k + 1)
                    px, kpx = pX[k % 2], ("pX", k % 2)
                    pt, kpt = pT[k % 3], ("pT", k % 3)
                    S.act(pt[:], px[:], AF.Exp, [kpx], [kpt])
                    S.tt("dve", pt[:].rearrange("p (h q) -> p h q", h=4), pt[:].rearrange("p (h q) -> p h q", h=4),
                         MT[b][:, j:j + 1, :].to_broadcast([128, 4, 128]), ALU.mult, [kpt, kmt_], [kpt])
                    S.mm(pO[:], ckvt[:, j, :], pt[:], j == 0, j == i, ["ckvt", kpt], ["pO"])
                    S.mm(pR[:], C.onesb[:], pt[:], j == 0, j == i, ["const", kpt], ["pR"])
                    if j == i:
                        S.act(rinv[:], pR[:], AF.Ln, ["pR"], ["rinv"])
                        S.act(rinv[:], rinv[:], AF.Exp, ["rinv"], ["rinv"], scale=-1.0)
                        S.tt("dve", olT[:, hg * 4:(hg + 1) * 4, ts_c], pO[:].rearrange("p (h q) -> p h q", h=4),
                             rinv[:].rearrange("p (h q) -> p h q", h=4), ALU.mult, ["pO", "rinv"], ["olT"])
                    yield

            gens = [gen_B()]
            if i > 0:
                gens.append(gen_C(i - 1, 1 - b))
            while gens:
                for g_ in list(gens):
                    try:
                        next(g_)
                    except StopIteration:
                        gens.remove(g_)
            kmk = "Mk"
            S.ts("dve", Mk[b][:, 0:nk], score[b][:, 0:nk], B[:, 0:1], None, ALU.is_ge, None, [ksc, kbs], [kmk])
            kmt = ("MT", b)
            for j0 in range(0, i + 1, 8):
                nj = min(8, i + 1 - j0)
                for j in range(j0, j0 + nj):
                    S.tr(pMT[:, (j - j0) * 128:(j - j0 + 1) * 128], Mk[b][:, j * 128:(j + 1) * 128], C.identb[:],
                         [kmk], ["pMT"])
                S.cp("act", MT[b][:, j0:j0 + nj, :], pMT[:, 0:nj * 128].rearrange("p (j q) -> p j q", j=nj),
                     ["pMT"], [kmt])
            if i == NT - 1:
                for _ in gen_C(i, b):
                    pass
        S.barrier()


def phase_dn(C, l):
    nc, S = C.nc, C.S
    xT, ydT = C.xT, C.ydT
    with ExitStack() as es:
        A = lambda n, sh, dt: es.enter_context(nc.sbuf_tensor(uid(n), sh, dt))
        P = lambda n, sh, dt: es.enter_context(nc.psum_tensor(uid(n), sh, dt))
        w_f = A("w_f", [128, 8, 1536], BF16)
        w_t = A("w_t", [128, 8, 520], BF16)
        convw = A("convw", [128, 48], F32)
        alog = A("alog", [128, 4], F32)
        dtb = A("dtb", [128, 4], F32)
        nrm = A("nrm", [128, 128], F32)
        Sst = A("Sst", [128, 4, 128], F32)
        xs = [A("xs", [128, 12, 131], F32) for _ in range(2)]
        cv = [A("cv", [128, 12, 128], F32)] * 2
        sq = A("sq", [128, 8, 128], F32)
        ctmp = A("ctmp", [128, 6, 128], F32)
        qT = [A("qT", [128, 4, 128], F32)] * 2
        kT = [A("kT", [128, 4, 128], F32)] * 2
        ktok = [A("ktok", [128, 4, 128], F32)] * 2
        vtok = [A("vtok", [128, 4, 128], F32)] * 2
        pjt = [A("pjt", [128, 520], F32)] * 2
        sz = [A("sz", [128, 512], F32)] * 2
        sm = [A("sm", [128, 48], F32)] * 2
        Gb = [A("Gb", [128, 4, 128], F32)] * 2
        Dm = [A("Dm", [128, 4, 128], F32)] * 2
        Ds = [A("Ds", [128, 4, 128], F32)] * 2
        Pm = [A("Pm", [128, 4, 128], F32) for _ in range(2)]
        PmT = [A("PmT", [128, 4, 128], F32) for _ in range(2)]
        Tt = [A("Tt", [128, 4, 128], F32) for _ in range(2)]
        Aq = A("Aq", [128, 4, 128], F32)
        gB = A("gB", [128, 4, 128], F32)
        AqT = A("AqT", [128, 4, 128], F32)
        kbg = A("kbg", [128, 4, 128], F32)
        kd = A("kd", [128, 4, 128], F32)
        vb = A("vb", [128, 4, 128], F32)
        wT = A("wT", [128, 4, 128], F32)
        vn = A("vn", [128, 4, 128], F32)
        t1 = A("t1", [128, 4, 128], F32)
        oo = A("oo", [128, 4, 128], F32)
        yd = A("yd", [128, 512], BF16)
        p0 = P("p0", [128, 512], F32)
        p1 = P("p1", [128, 512], F32)
        p2 = P("p2", [128, 512], F32)
        p3 = P("p3", [128, 512], F32)
        p4 = P("p4", [128, 512], F32)
        p5 = P("p5", [128, 512], F32)
        pTb = P("pTb", [128, 1024], BF16)

        wv = C.w_in[l].rearrange("(c p) n -> p c n", p=128)
        S.dma("pool", w_f[:, :, 0:768], wv[:, :, 0:768], [], ["w_f"])
        S.dma("pool", w_f[:, :, 768:1536], wv[:, :, 768:1536], [], ["w_f"])
        S.dma("pool", w_t[:], wv[:, :, 1536:2056], [], ["w_t"])
        S.dma("sp", convw[:], C.convw[l], [], ["convw"])
        S.dma("sp", alog[:], C.alog[l], [], ["alog"])
        S.dma("sp", dtb[:], C.dtb[l], [], ["dtb"])
        S.dma("sp", nrm[:], C.dnnorm[l], [], ["nrm"])
        S.op("dve", lambda e: e.memset(Sst[:], 0.0), [], ["Sst"])
        S.op("dve", lambda e: e.memset(xs[1][:, :, 128:131], 0.0), [], [("xs", 1)])
        S.act(alog[:], alog[:], AF.Exp, ["alog"], ["alog"])
        S.ts("dve", alog[:], alog[:], -1.0, None, ALU.mult, None, ["alog"], ["alog"])

        def h4(t):
            return t[:].rearrange("p h c -> p (h c)")

        for i in range(NT):
            b = i % 2
            ts_ = slice(i * 128, (i + 1) * 128)
            kxs, kxp = ("xs", b), ("xs", 1 - b)
            S.cp("dve", xs[b][:, :, 0:3], xs[1 - b][:, :, 128:131], [kxp], [kxs])
            for g3 in range(3):
                pp, pk = [(p0, "p0"), (p1, "p1"), (p2, "p2")][g3]
                for c4 in range(4):
                    ct = g3 * 4 + c4
                    for dc in range(8):
                        S.mm(pp[:, c4 * 128:(c4 + 1) * 128], w_f[:, dc, ct * 128:(ct + 1) * 128], xT[:, dc, ts_],
                             dc == 0, dc == 7, ["w_f", "xT"], [pk])
                S.cp("act", xs[b][:, g3 * 4:(g3 + 1) * 4, 3:131], pp[:].rearrange("p (c t) -> p c t", c=4), [pk], [kxs])
            for (c0, c1, pp, pk) in [(0, 512, p3, "p3"), (512, 520, p4, "p4")]:
                for dc in range(8):
                    S.mm(pp[:, 0:c1 - c0], xT[:, dc, ts_], w_t[:, dc, c0:c1], dc == 0, dc == 7, ["xT", "w_t"], [pk])
            kpj = "pjt_"
            ksz = "sz_"
            S.act(sz[b][:], p3[:, 0:512], AF.Silu, ["p3"], [ksz])
            S.cp("dve", pjt[b][:, 512:520], p4[:, 0:8], ["p4"], [kpj])
            kcv = "cv_"
            cwv = convw[:].rearrange("p (c k) -> p c k", k=4)
            for h6 in range(2):
                cs6 = slice(h6 * 6, (h6 + 1) * 6)
                S.tt("dve", cv[b][:, cs6, :], xs[b][:, cs6, 3:131], cwv[:, cs6, 3:4].to_broadcast([128, 6, 128]),
                     ALU.mult, [kxs, "convw"], [kcv])
                for k in range(3):
                    S.tt("dve", ctmp[:], xs[b][:, cs6, k:k + 128], cwv[:, cs6, k:k + 1].to_broadcast([128, 6, 128]),
                         ALU.mult, [kxs, "convw"], ["ctmp"])
                    S.tt("dve", cv[b][:, cs6, :], cv[b][:, cs6, :], ctmp[:], ALU.add, [kcv, "ctmp"], [kcv])
            S.act(cv[b][:], cv[b][:], AF.Silu, [kcv], [kcv])
            S.act(sq[:], cv[b][:, 0:8, :], AF.Square, [kcv], ["sq"])
            for hh in range(2):
                pp, pk = [(p0, "p0"), (p1, "p1")][hh]
                S.mm(pp[:], C.ones[:], sq[:, hh * 4:(hh + 1) * 4, :].rearrange("p h c -> p (h c)"), True, True,
                     ["const", "sq"], [pk])
                S.act(sq[:, hh * 4:(hh + 1) * 4, :].rearrange("p h c -> p (h c)"), pp[:], AF.Ln, [pk], ["sq"],
                      bias=C.eps[:, 0:1])
            S.act(sq[:], sq[:], AF.Exp, ["sq"], ["sq"], scale=-0.5)
            kq, kk = "qT_", "kT_"
            S.stt("dve", qT[b][:], cv[b][:, 0:4, :], float(128 ** -0.5), sq[:, 0:4, :], ALU.mult, ALU.mult,
                  [kcv, "sq"], [kq])
            S.tt("dve", kT[b][:], cv[b][:, 4:8, :], sq[:, 4:8, :], ALU.mult, [kcv, "sq"], [kk])
            for hh in range(4):
                S.tr(p0[:, hh * 128:(hh + 1) * 128], kT[b][:, hh, :], C.ident[:], [kk], ["p0"])
                S.tr(p1[:, hh * 128:(hh + 1) * 128], cv[b][:, 8 + hh, :], C.ident[:], [kcv], ["p1"])
            kkt, kvt = "ktok_", "vtok_"
            S.cp("act", h4(ktok[b]), p0[:], ["p0"], [kkt])
            S.cp("act", h4(vtok[b]), p1[:], ["p1"], [kvt])
            ksm = "sm_"
            M_ = sm[b]
            S.act(M_[:, 0:4], pjt[b][:, 512:516], AF.Sigmoid, [kpj], [ksm])
            S.tt("dve", M_[:, 24:28], pjt[b][:, 516:520], dtb[:], ALU.add, [kpj, "dtb"], [ksm])
            S.act(M_[:, 28:32], M_[:, 24:28], AF.Abs, [ksm], [ksm])
            S.act(M_[:, 28:32], M_[:, 28:32], AF.Exp, [ksm], [ksm], scale=-1.0)
            S.act(M_[:, 28:32], M_[:, 28:32], AF.Ln, [ksm], [ksm], bias=C.ones[:, 0:1])
            S.stt("dve", M_[:, 24:28], M_[:, 24:28], 0.0, M_[:, 28:32], ALU.max, ALU.add, [ksm], [ksm])
            S.tt("dve", M_[:, 4:8], M_[:, 24:28], alog[:], ALU.mult, [ksm, "alog"], [ksm])
            S.mm(p2[:, 0:4], C.triu[:], M_[:, 4:8], True, True, ["const", ksm], ["p2"])
            kgb = "Gb_"
            S.cp("dve", gB[:], M_[:, 4:8].unsqueeze(2).to_broadcast([128, 4, 128]), [ksm], ["gB"])
            for hh in range(4):
                S.mm(p3[:, hh * 128:(hh + 1) * 128], gB[:, hh, :], C.triu[:], True, True, ["gB", "const"], ["p3"])
            S.cp("dve", M_[:, 8:12], p2[:, 0:4], ["p2"], [ksm])
            S.cp("act", h4(Gb[b]), p3[:], ["p3"], [kgb])
            S.act(M_[:, 12:16], M_[:, 8:12], AF.Exp, [ksm], [ksm])
            S.tt("dve", M_[:, 16:20], M_[:, 12:16], M_[:, 0:4], ALU.mult, [ksm], [ksm])
            S.tt("dve", M_[:, 20:24], Gb[b][:, :, 127], M_[:, 8:12], ALU.subtract, [kgb, ksm], [ksm])
            S.act(M_[:, 20:24], M_[:, 20:24], AF.Exp, [ksm], [ksm])
            S.act(M_[:, 32:36], Gb[b][:, :, 127], AF.Exp, [kgb], [ksm])
            S.ts("dve", M_[:, 36:40], M_[:, 0:4], -1.0, None, ALU.mult, None, [ksm], [ksm])
            kdm, kds = "Dm_", "Ds_"
            for hh in range(4):
                S.ts("dve", Dm[b][:, hh, :], Gb[b][:, hh, :], M_[:, 8 + hh:9 + hh], 0.0, ALU.subtract, ALU.max,
                     [kgb, ksm], [kdm])
            S.act(h4(Dm[b]), h4(Dm[b]), AF.Exp, [kdm], [kdm], scale=-1.0)
            S.tt("pool", Ds[b][:], Dm[b][:], C.trils[:].unsqueeze(1).to_broadcast([128, 4, 128]), ALU.mult,
                 [kdm, "const"], [kds])
            S.tt("pool", Dm[b][:], Dm[b][:], C.tril[:].unsqueeze(1).to_broadcast([128, 4, 128]), ALU.mult,
                 [kdm, "const"], [kdm])
            for hh in range(4):
                S.mm(p0[:, hh * 128:(hh + 1) * 128], kT[b][:, hh, :], kT[b][:, hh, :], True, True, [kk], ["p0"])
                S.mm(p1[:, hh * 128:(hh + 1) * 128], qT[b][:, hh, :], kT[b][:, hh, :], True, True, [kq, kk], ["p1"])
            kpm, kpmt, ktt = ("Pm", 0), ("PmT", 0), ("Tt", 0)
            for hh in range(4):
                S.stt("dve", Pm[0][:, hh, :], p0[:, hh * 128:(hh + 1) * 128], M_[:, 36 + hh:37 + hh], Ds[b][:, hh, :],
                      ALU.mult, ALU.mult, ["p0", ksm, kds], [kpm])
            S.tt("dve", h4(Aq), p1[:], h4(Dm[b]), ALU.mult, ["p1", kdm], ["Aq"])
            for hh in range(4):
                S.tr(p2[:, hh * 128:(hh + 1) * 128], Pm[0][:, hh, :], C.ident[:], [kpm], ["p2"])
                S.tr(p3[:, hh * 128:(hh + 1) * 128], Aq[:, hh, :], C.ident[:], ["Aq"], ["p3"])
            S.cp("act", h4(PmT[0]), p2[:], ["p2"], [kpmt])
            S.cp("act", h4(AqT), p3[:], ["p3"], ["AqT"])
            S.tt("dve", Tt[0][:], PmT[0][:], C.ident[:].unsqueeze(1).to_broadcast([128, 4, 128]), ALU.add,
                 [kpmt, "const"], [ktt])
            cur = 0
            for lev in range(1, 7):
                nxt = 1 - cur
                kpm_n, kpmt_n, ktt_n = ("Pm", nxt), ("PmT", nxt), ("Tt", nxt)
                kpm_c, kpmt_c, ktt_c = ("Pm", cur), ("PmT", cur), ("Tt", cur)
                for hh in range(4):
                    S.mm(p0[:, hh * 128:(hh + 1) * 128], PmT[cur][:, hh, :], Pm[cur][:, hh, :], True, True,
                         [kpm_c, kpmt_c], ["p0"])
                    if lev < 6:
                        S.mm(p1[:, hh * 128:(hh + 1) * 128], Pm[cur][:, hh, :], PmT[cur][:, hh, :], True, True,
                             [kpm_c, kpmt_c], ["p1"])
                S.cp("act", h4(Pm[nxt]), p0[:], ["p0"], [kpm_n])
                if lev < 6:
                    S.cp("dve", h4(PmT[nxt]), p1[:], ["p1"], [kpmt_n])
                for hh in range(4):
                    S.mm(p2[:, hh * 128:(hh + 1) * 128], Pm[nxt][:, hh, :], Tt[cur][:, hh, :], True, True,
                         [kpm_n, ktt_c], ["p2"])
                S.tt("dve", h4(Tt[nxt]), p2[:], h4(Tt[cur]), ALU.add, ["p2", ktt_c], [ktt_n])
                cur = nxt
            ktt = ("Tt", cur)
            TT = Tt[cur]
            for hh in range(4):
                S.ts("pool", kbg[:, hh, :], ktok[b][:, hh, :], M_[:, 16 + hh:17 + hh], None, ALU.mult, None,
                     [kkt, ksm], ["kbg"])
                S.ts("pool", kd[:, hh, :], ktok[b][:, hh, :], M_[:, 20 + hh:21 + hh], None, ALU.mult, None,
                     [kkt, ksm], ["kd"])
                S.ts("pool", vb[:, hh, :], vtok[b][:, hh, :], M_[:, 0 + hh:1 + hh], None, ALU.mult, None,
                     [kvt, ksm], ["vb"])
            for hh in range(4):
                S.mm(p0[:, hh * 128:(hh + 1) * 128], kbg[:, hh, :], TT[:, hh, :], True, True, ["kbg", ktt], ["p0"])
            S.act(h4(wT), p0[:], AF.Copy, ["p0"], ["wT"], scale=-1.0)
            for hh in range(4):
                S.mm(p1[:, hh * 128:(hh + 1) * 128], TT[:, hh, :], vb[:, hh, :], True, False, [ktt, "vb"], ["p1"])
                S.mm(p1[:, hh * 128:(hh + 1) * 128], wT[:, hh, :], Sst[:, hh, :], False, True, ["wT", "Sst"], ["p1"])
            S.cp("act", h4(vn), p1[:], ["p1"], ["vn"])
            for hh in range(4):
                S.mm(p2[:, hh * 128:(hh + 1) * 128], qT[b][:, hh, :], Sst[:, hh, :], True, True, [kq, "Sst"], ["p2"])
                S.mm(p3[:, hh * 128:(hh + 1) * 128], AqT[:, hh, :], vn[:, hh, :], True, True, ["AqT", "vn"], ["p3"])
            for hh in range(4):
                S.act(t1[:, hh, :], p2[:, hh * 128:(hh + 1) * 128], AF.Copy, ["p2", ksm], ["t1"],
                      scale=M_[:, 12 + hh:13 + hh])
            S.tt("dve", h4(oo), p3[:], h4(t1), ALU.add, ["p3", "t1"], ["oo"])
            for hh in range(4):
                S.mm(p4[:, hh * 128:(hh + 1) * 128], kd[:, hh, :], vn[:, hh, :], True, True, ["kd", "vn"], ["p4"])
            for hh in range(4):
                S.stt("dve", Sst[:, hh, :], Sst[:, hh, :], M_[:, 32 + hh:33 + hh], p4[:, hh * 128:(hh + 1) * 128],
                      ALU.mult, ALU.add, ["Sst", ksm, "p4"], ["Sst"])
            for hh in range(4):
                S.act(t1[:, hh, :], oo[:, hh, :], AF.Square, ["oo"], ["t1", ksm], accum_out=M_[:, 40 + hh:41 + hh])
            S.act(M_[:, 40:44], M_[:, 40:44], AF.Sqrt, [ksm], [ksm], scale=1.0 / 128, bias=C.eps[:, 0:1])
            S.op("dve", lambda e: e.reciprocal(M_[:, 40:44], M_[:, 40:44]), [ksm], [ksm])
            for hh in range(4):
                S.stt("dve", oo[:, hh, :], oo[:, hh, :], M_[:, 40 + hh:41 + hh], nrm[:], ALU.mult, ALU.mult,
                      ["oo", ksm, "nrm"], ["oo"])
            S.tt("dve", yd[:], h4(oo), sz[b][:], ALU.mult, ["oo", ksz], ["yd"])
            for hh in range(4):
                S.tr(pTb[:, hh * 128:(hh + 1) * 128], yd[:, hh * 128:(hh + 1) * 128], C.identb[:], ["yd"], ["pTb"])
            S.cp("act", ydT[:, :, ts_], pTb[:, 0:512].rearrange("p (h c) -> p h c", h=4), ["pTb"], ["ydT"])
        S.barrier()


def layer_norm_tile(C, S, src, dst, g_bc, b_bc, st, kst, rsrc, kdst, junk):
    S.act(junk, src, AF.Identity, rsrc, ["lnjunk", kst], accum_out=st[:, 0:1], scale=1.0 / D)
    S.ts("dve", src, src, st[:, 0:1], None, ALU.subtract, None, rsrc + [kst], rsrc)
    S.act(junk, src, AF.Square, rsrc, ["lnjunk", kst], accum_out=st[:, 1:2])
    S.act(st[:, 1:2], st[:, 1:2], AF.Sqrt, [kst], [kst], scale=1.0 / D, bias=C.eps[:, 0:1])
    S.op("dve", lambda e: e.reciprocal(st[:, 1:2], st[:, 1:2]), [kst], [kst])
    S.stt("dve", src, src, st[:, 1:2], g_bc, ALU.mult, ALU.mult, rsrc + [kst, "lng"], rsrc)
    S.tt("pool", dst, src, b_bc, ALU.add, rsrc + ["lnb"], [kdst])


def phase_c(C, l, x_src, x_dst):
    nc, S = C.nc, C.S
    with ExitStack() as es:
        A = lambda n, sh, dt: es.enter_context(nc.sbuf_tensor(uid(n), sh, dt))
        P = lambda n, sh, dt: es.enter_context(nc.psum_tensor(uid(n), sh, dt))
        wod = A("wod", [128, 4, D], BF16)
        wos = A("wos", [64, 8, D], F32)
        wuvT = A("wuvT", [64, 8, 128], F32)
        Wvo = A("Wvo", [128, 8, D], BF16)
        lng = A("lng", [128, D], F32)
        lnb = A("lnb", [128, D], F32)
        rw = A("rw", [128, 8, NE], F32)
        rb = A("rb", [128, NE], F32)
        xin = [A("xin", [128, D], F32) for _ in range(2)]
        xo = [A("xo", [128, D], F32) for _ in range(2)]
        xTf = [A("xTf", [128, 8, 128], F32) for _ in range(2)]
        junk = A("lnjunk", [128, D], F32)
        st = [A("lst", [128, 8], F32) for _ in range(2)]
        lg = [A("lg", [128, NE], F32) for _ in range(2)]
        top8 = [A("top8", [128, 8], F32) for _ in range(2)]
        ex = [A("ex", [128, NE], F32) for _ in range(2)]
        pm = [P("pm", [128, 512], F32) for _ in range(2)]
        ptr = [P("ptr", [128, 512], F32) for _ in range(2)]
        plg = P("plg", [128, 512], F32)

        S.dma("pool", wod[:], C.w_o[l, 0:512, :].rearrange("(c p) n -> p c n", p=128), [], ["wod"])
        S.dma("sp", wos[:], C.w_o[l, 512:1024, :].rearrange("(h v) n -> v h n", v=64), [], ["wos"])
        S.dma("sp", wuvT[:], C.wuvT[l].rearrange("p (h r) -> p h r", h=8), [], ["wuvT"])
        S.dma("sp", lng[:], C.ln1g[l], [], ["lng"])
        S.dma("sp", lnb[:], C.ln1b[l], [], ["lnb"])
        S.dma("sp", rw[:], C.router_w[l].rearrange("(c p) n -> p c n", p=128), [], ["rw"])
        S.dma("sp", rb[:], C.router_b[l], [], ["rb"])
        for h in range(8):
            for dh in range(2):
                S.mm(pm[dh][:], wuvT[:, h, :], wos[:, h, dh * 512:(dh + 1) * 512], True, True, ["wuvT", "wos"],
                     [("pm", dh)])
                S.cp("act" if dh == 0 else "dve", Wvo[:, h, dh * 512:(dh + 1) * 512], pm[dh][:], [("pm", dh)], ["Wvo"])

        for i in range(NT):
            b = i % 2
            ts_ = slice(i * 128, (i + 1) * 128)
            kxi, kxo = ("xin", b), ("xo", b)
            S.dma("sp", xin[b][:], x_src[ts_, :], [("xsrc", i)], [kxi])
            for dh in range(2):
                ds_ = slice(dh * 512, (dh + 1) * 512)
                for c in range(4):
                    S.mm(pm[dh][:], C.ydT[:, c, ts_], wod[:, c, ds_], c == 0, False, ["ydT", "wod"], [("pm", dh)])
                for h in range(8):
                    S.mm(pm[dh][:], C.olT[:, h, ts_], Wvo[:, h, ds_], False, h == 7, ["olT", "Wvo"], [("pm", dh)])
                S.stt("dve", xin[b][:, ds_], xin[b][:, ds_], ALPHA, pm[dh][:], ALU.mult, ALU.add,
                      [kxi, ("pm", dh)], [kxi])
            kst = ("lst", b)
            layer_norm_tile(C, S, xin[b][:], xo[b][:], lng[:], lnb[:], st[b], kst, [kxi], kxo, junk[:])
            S.dma("sp", x_dst[ts_, :], xo[b][:], [kxo], [("xdst", i)], semkey="xdst")
            kxf = ("xTf", b)
            for hf in range(2):
                for c4 in range(4):
                    dc = hf * 4 + c4
                    S.tr(ptr[hf][:, c4 * 128:(c4 + 1) * 128], xo[b][:, dc * 128:(dc + 1) * 128], C.ident[:], [kxo],
                         [("ptr", hf)])
                S.cp("act", xTf[b][:, hf * 4:(hf + 1) * 4, :], ptr[hf][:].rearrange("p (c t) -> p c t", c=4),
                     [("ptr", hf)], [kxf])
                S.cp("dve", C.xT[:, hf * 4:(hf + 1) * 4, ts_], xTf[b][:, hf * 4:(hf + 1) * 4, :], [kxf], ["xT"])
            for dc in range(8):
                S.mm(plg[:, 0:NE], xTf[b][:, dc, :], rw[:, dc, :], dc == 0, dc == 7, [kxf, "rw"], ["plg"])
            klg = ("lg", b)
            S.tt("dve", lg[b][:], plg[:, 0:NE], rb[:], ALU.add, ["plg", "rb"], [klg])
            S.op("dve", lambda e: e.max(out=top8[b][:], in_=lg[b][:]), [klg], [("top8", b)])
            S.ts("dve", ex[b][:], lg[b][:], top8[b][:, 0:1], None, ALU.subtract, None, [klg, ("top8", b)], [("ex", b)])
            S.act(ex[b][:], ex[b][:], AF.Exp, [("ex", b)], [("ex", b)])
            S.ts("dve", lg[b][:], lg[b][:], top8[b][:, 3:4], None, ALU.is_ge, None, [klg, ("top8", b)], [klg])
            S.tt("dve", ex[b][:], ex[b][:], lg[b][:], ALU.mult, [("ex", b), klg], [("ex", b)])
            S.op("dve", lambda e: e.tensor_reduce(st[b][:, 4:5], ex[b][:], AX.X, ALU.add), [("ex", b)], [kst])
            S.op("dve", lambda e: e.reciprocal(st[b][:, 4:5], st[b][:, 4:5]), [kst], [kst])
            S.ts("dve", C.cw[:, i, :], ex[b][:], st[b][:, 4:5], None, ALU.mult, None, [("ex", b), kst], ["cw"])
            S.tr(plg[0:NE, 128:256], C.cw[:, i, :], C.ident[:], ["cw"], ["plg"])
            S.cp("act", C.cwT[:, ts_], plg[0:NE, 128:256], ["plg"], ["cwT"])
        S.barrier()


def phase_moe(C, l, x_src, x_dst, last):
    nc, S = C.nc, C.S
    xT, cw, cwT = C.xT, C.cw, C.cwT
    with ExitStack() as es:
        A = lambda n, sh, dt: es.enter_context(nc.sbuf_tensor(uid(n), sh, dt))
        P = lambda n, sh, dt: es.enter_context(nc.psum_tensor(uid(n), sh, dt))
        X = A("X", [128, NT, D], F32)
        NB = 2
        Wg = [A("Wg", [128, 8, 256], BF16) for _ in range(NB)]
        Wu = [A("Wu", [128, 8, 256], BF16) for _ in range(NB)]
        Wd = [A("Wd", [128, 2, D], BF16) for _ in range(NB)]
        bgu = A("bgu", [128, NE * 16], F32)
        bdn = A("bdn", [NE, D], F32)
        lng = A("lng", [128, D], F32)
        lnb = A("lnb", [128, D], F32)
        hT = [A("hT", [128, 2, 512], BF16) for _ in range(2)]
        gt = [A("gt", [128, 512], F32) for _ in range(2)]
        sg = [A("sg", [128, 512], F32) for _ in range(2)]
        ut = [A("ut", [128, 512], F32) for _ in range(2)]
        junk = A("lnjunk", [128, D], F32)
        xo = [A("xo", [128, D], F32) for _ in range(2)]
        st = [A("lst", [128, 8], F32) for _ in range(2)]
        pg = [P("pg", [128, 512], F32) for _ in range(2)]
        pu = [P("pu", [128, 512], F32) for _ in range(2)]
        po = [P("po", [128, 512], F32) for _ in range(4)]

        S.dma("sp", bgu[:], C.bgu[l], [], ["bgu"])
        S.dma("sp", bdn[:], C.bdn[l], [], ["bdn"])
        bg2 = A("bg2", [128, NE * 16], F32)
        c119 = A("c119", [128, 1], F32)
        S.op("dve", lambda e: e.memset(c119[:], 1.702 * 7.0), [], ["bg2"])
        bv = bgu[:].rearrange("p (e c) -> p e c", c=16)
        b2v = bg2[:].rearrange("p (e c) -> p e c", c=16)
        S.ts("dve", b2v[:, :, 0:8], bv[:, :, 0:8], -1.0, 7.0, ALU.mult, ALU.add, ["bgu"], ["bg2"])
        S.ts("dve", b2v[:, :, 8:16], bv[:, :, 8:16], 7.0, None, ALU.add, None, ["bgu"], ["bg2"])
        S.dma("sp", lng[:], C.ln2g[l], [], ["lng"])
        S.dma("sp", lnb[:], C.ln2b[l], [], ["lnb"])
        for i in range(NT):
            S.dma("sp", X[:, i, :], x_src[i * 128:(i + 1) * 128, :], [("xdst", i)], [("X", i)])

        steps = [(e, hf) for e in range(NE) for hf in range(4)]

        def load_w(si, parts=(0, 1, 2)):
            e, hf = steps[si]
            bi = si % NB
            gv = C.wgu[l, e].rearrange("(c p) n -> p c n", p=128)
            if 0 in parts:
                S.dma("pool", Wg[bi][:], gv[:, :, hf * 256:(hf + 1) * 256], [], [("Wg", bi)])
            if 1 in parts:
                S.dma("pool", Wu[bi][:], gv[:, :, 1024 + hf * 256:1024 + (hf + 1) * 256], [], [("Wu", bi)])
            if 2 in parts:
                S.dma("pool", Wd[bi][:], C.wdn[l, e, hf * 256:(hf + 1) * 256, :].rearrange("(c p) n -> p c n", p=128),
                      [], [("Wd", bi)])

        load_w(0)
        for i in range(NT):
            for dh in range(2):
                ds_ = slice(dh * 512, (dh + 1) * 512)
                S.mm(po[dh][:], cwT[:, i * 128:(i + 1) * 128], bdn[:, ds_], True, True, ["cwT", "bdn"], [("po", dh)])
                S.stt("dve", X[:, i, ds_], X[:, i, ds_], ALPHA, po[dh][:], ALU.mult, ALU.add,
                      [("X", i), ("po", dh)], [("X", i, dh)])

        work = [(si, tg) for si in range(len(steps)) for tg in range(4)]

        acc_t = [A("acct", [128, 512], F32) for _ in range(4)]

        def up_mm(wi, fc):
            si, tg = work[wi]
            bi = si % NB
            tsl = slice(tg * 512, (tg + 1) * 512)
            pb_ = fc
            fs = slice(fc * 128, (fc + 1) * 128)
            for dc in range(8):
                S.mm(pg[pb_][:], Wg[bi][:, dc, fs], xT[:, dc, tsl], dc == 0, dc == 7, [("Wg", bi), "xT"],
                     [("pg", pb_)])
            for dc in range(8):
                S.mm(pu[pb_][:], Wu[bi][:, dc, fs], xT[:, dc, tsl], dc == 0, dc == 7, [("Wu", bi), "xT"],
                     [("pu", pb_)])

        def up_epi(wi, fc):
            si, tg = work[wi]
            e, hf = steps[si]
            hb = wi % 2
            pb_ = fc
            cg = e * 16 + hf * 2 + fc
            cu = e * 16 + 8 + hf * 2 + fc
            kg, ksg, ku = ("gt", pb_), ("sg", pb_), ("ut", pb_)
            S.act(gt[pb_][:], pg[pb_][:], AF.Relu, [("pg", pb_), "bg2"], [kg], scale=-1.0, bias=bg2[:, cg:cg + 1])
            S.act(sg[pb_][:], gt[pb_][:], AF.Sigmoid, [kg, "bg2"], [ksg], scale=-1.702, bias=c119[:, 0:1])
            S.act(ut[pb_][:], pu[pb_][:], AF.Relu, [("pu", pb_), "bg2"], [ku], bias=bg2[:, cu:cu + 1])
            S.stt("dve", gt[pb_][:], gt[pb_][:], 7.0, sg[pb_][:], ALU.subtract, ALU.mult, [kg, ksg], [kg])
            S.ts("dve", ut[pb_][:], ut[pb_][:], 14.0, -6.0, ALU.min, ALU.add, [ku], [ku])
            S.stt("dve", hT[hb][:, fc, :], ut[pb_][:], -1.0, gt[pb_][:], ALU.mult, ALU.mult, [ku, kg], [("hT", hb)])

        def down(wi, tts):
            si, tg = work[wi]
            e, hf = steps[si]
            bi = si % NB
            hb = wi % 2
            for tt in tts:
                i = tg * 4 + tt
                for dh in range(2):
                    ds_ = slice(dh * 512, (dh + 1) * 512)
                    pi = dh * 2 + tt % 2
                    for fc in range(2):
                        S.mm(po[pi][:], hT[hb][:, fc, tt * 128:(tt + 1) * 128], Wd[bi][:, fc, ds_], fc == 0, fc == 1,
                             [("hT", hb), ("Wd", bi)], [("po", pi)])
                    kx = ("X", i, dh)
                    if dh == 0:
                        S.stt("dve", X[:, i, ds_], po[pi][:], cw[:, i, e:e + 1], X[:, i, ds_], ALU.mult, ALU.add,
                              [("po", pi), "cw", kx], [kx])
                    else:
                        ab = tt
                        S.act(acc_t[ab][:], po[pi][:], AF.Copy, [("po", pi), "cw"], [("acct", ab)],
                              scale=cw[:, i, e:e + 1])
                        S.tt("pool", X[:, i, ds_], X[:, i, ds_], acc_t[ab][:], ALU.add, [("acct", ab), kx], [kx])

        nW = len(work)
        for fc in range(2):
            up_mm(0, fc)
            up_epi(0, fc)
        for wi in range(nW):
            si, tg = work[wi]
            for half in range(2):
                part = tg * 2 + half
                if part < 3 and si + 1 < len(steps):
                    load_w(si + 1, (part,))
                if wi + 1 < nW:
                    up_mm(wi + 1, half)
                down(wi, [2 * half, 2 * half + 1])
                if wi + 1 < nW:
                    up_epi(wi + 1, half)

        ptm = pg[0][:].bitcast(BF16)
        for i in range(NT):
            b = i % 2
            ts_ = slice(i * 128, (i + 1) * 128)
            kst = ("lst", b)
            kxo = ("xo", b)
            layer_norm_tile(C, S, X[:, i, :], xo[b][:], lng[:], lnb[:], st[b], kst, [("X", i, 0), ("X", i, 1)], kxo, junk[:])
            S.dma("sp", x_dst[ts_, :], xo[b][:], [kxo], [("xsrc", i)], semkey="xout")
            if not last:
                S.cp("pool", C.xb[:], xo[b][:], [kxo], ["xb"])
                for c in range(8):
                    S.tr(ptm[:, c * 128:(c + 1) * 128], C.xb[:, c * 128:(c + 1) * 128], C.identb[:], ["xb"], [("pg", 0)])
                S.cp("act", C.xT[:, :, ts_], ptm[:].rearrange("p (c t) -> p c t", c=8), [("pg", 0)], ["xT"])
        S.barrier()


def phase_in(C, x_src):
    nc, S = C.nc, C.S
    with ExitStack() as es:
        A = lambda n, sh, dt: es.enter_context(nc.sbuf_tensor(uid(n), sh, dt))
        xin = [A("xin", [128, D], BF16) for _ in range(2)]
        pt_ = [es.enter_context(nc.psum_tensor(uid("pti"), [128, 1024], BF16)) for _ in range(2)]
        for i in range(NT):
            b = i % 2
            ts_ = slice(i * 128, (i + 1) * 128)
            S.dma("pool", xin[b][:], x_src[ts_, :], [], [("xin", b)])
            for c in range(8):
                S.tr(pt_[b][:, c * 128:(c + 1) * 128], xin[b][:, c * 128:(c + 1) * 128], C.identb[:], [("xin", b)],
                     [("pti", b)])
            S.cp("act" if b else "dve", C.xT[:, :, ts_], pt_[b][:].rearrange("p (c t) -> p c t", c=8), [("pti", b)],
                 ["xT"])
        S.barrier()


def dbg_dump(C, dbg, x1):
    if not dbg:
        return
    nc, S = C.nc, C.S
    for nm, t in [("d_olT", getattr(C, "olT", None)), ("d_ydT", getattr(C, "ydT", None)), ("d_xT", C.xT)]:
        if t is None:
            continue
        d = nc.dram_tensor(nm, list(t.shape), BF16, kind="ExternalOutput").ap()
        S.dma("sp", d, t[:], [], [nm])
    d = nc.dram_tensor("d_x1", [T, D], F32, kind="ExternalOutput").ap()
    S.dma("sp", d, x1, [], ["d_x1"])
    d = nc.dram_tensor("d_cw", [128, NT, NE], F32, kind="ExternalOutput").ap()
    S.dma("sp", d, C.cw[:], [], ["d_cw"])
    S.barrier()


def build(nl=NL, l0=0, stop=None, dbg=False):
    _, C0 = _build(nl, l0, stop, dbg, None)
    return _build(nl, l0, stop, dbg, C0.S.needed)


def _build(nl, l0, stop, dbg, plan):
    nc = bass.Bass("TRN2", target_bir_lowering=False)
    C = Ctx()
    C.nc = nc

    def din(name, shape):
        return nc.dram_tensor(name, shape, F32, kind="ExternalInput").ap()

    C.x = din("x", [T, D])
    C.w_in = din("w_in", [NL, D, 2512])
    C.convw = din("convw", [NL, 128, 48])
    C.alog = din("alog", [NL, 128, 4])
    C.dtb = din("dtb", [NL, 128, 4])
    C.dnnorm = din("dnnorm", [NL, 128, 128])
    C.qn = din("qn", [NL, 128, 256])
    C.kvn = din("kvn", [NL, 128, 128])
    C.ig = din("ig", [NL, 128, 64])
    C.ib = din("ib", [NL, 128, 64])
    C.wuqT = din("wuqT", [NL, 64, 8 * 256])
    C.wuk = din("wuk", [NL, 64, 8 * 128])
    C.wuvT = din("wuvT", [NL, 64, 8 * 128])
    C.wqidx = din("wqidx", [NL, 256, 512])
    C.w_o = din("w_o", [NL, D, D])
    C.ln1g = din("ln1g", [NL, 128, D])
    C.ln1b = din("ln1b", [NL, 128, D])
    C.ln2g = din("ln2g", [NL, 128, D])
    C.ln2b = din("ln2b", [NL, 128, D])
    C.router_w = din("router_w", [NL, D, NE])
    C.router_b = din("router_b", [NL, 128, NE])
    if stop is None:
        C.wgu = din("wgu", [NL, NE, D, 2048])
        C.bgu = din("bgu", [NL, 128, NE * 16])
        C.wdn = din("wdn", [NL, NE, D, D])
        C.bdn = din("bdn", [NL, NE, D])
    out = nc.dram_tensor("out", [T, D], F32, kind="ExternalOutput").ap()
    xa = nc.dram_tensor("xa_scr", [T, D], F32, kind="Internal").ap()
    x1 = nc.dram_tensor("x1_scr", [T, D], F32, kind="Internal").ap()
    dbg_t = {}
    with ExitStack() as es:
        S = Sched(nc, es, plan)
        C.S = S
        setup_consts(C, es)
        C.xT = es.enter_context(nc.sbuf_tensor("xT", [128, 8, T], BF16))
        C.cw = es.enter_context(nc.sbuf_tensor("cw", [128, NT, NE], F32))
        C.cwT = es.enter_context(nc.sbuf_tensor("cwT", [NE, T], F32))
        C.xb = es.enter_context(nc.sbuf_tensor("xb", [128, D], BF16))
        S.barrier()
        phase_in(C, C.x)
        xsrc = C.x
        for li in range(nl):
            l = l0 + li
            last = li == nl - 1
            with ExitStack() as es2:
                C.olT = es2.enter_context(nc.sbuf_tensor(uid("olT"), [128, 8, T], BF16))
                phase_dsa(C, l)
                if stop == "dsa":
                    dbg_dump(C, dbg, x1)
                    break
                C.ydT = es2.enter_context(nc.sbuf_tensor(uid("ydT"), [128, 4, T], BF16))
                phase_dn(C, l)
                if stop == "dn":
                    dbg_dump(C, dbg, x1)
                    break
                phase_c(C, l, xsrc, x1)
                if stop == "c" or (dbg and last):
                    dbg_dump(C, dbg, x1)
                if stop == "c":
                    break
            xdst = out if last else xa
            phase_moe(C, l, x1, xdst, last)
            xsrc = xa
        S.barrier()
    C.ninst = S.ninst
    C.nwaits = S.nwaits
    return nc, C


def prep_inputs(inp):
    f = lambda a: np.ascontiguousarray(np.asarray(a, dtype=np.float32))
    rep = lambda a: f(np.broadcast_to(np.asarray(a)[:, None, :], (a.shape[0], 128, a.shape[1])))
    sh = {}
    sh["w_in"] = f(inp["w_in"])
    cw = np.asarray(inp["dn_conv"])
    sh["convw"] = f(cw.reshape(NL, 4, 12, 128).transpose(0, 3, 2, 1).reshape(NL, 128, 48))
    sh["alog"] = rep(inp["dn_a_log"])
    sh["dtb"] = rep(inp["dn_dt_bias"])
    sh["dnnorm"] = rep(inp["dn_norm"])
    sh["qn"] = rep(inp["sa_q_norm"])
    sh["kvn"] = rep(inp["sa_kv_norm"])
    sh["ig"] = rep(inp["idx_k_norm_g"])
    sh["ib"] = rep(inp["idx_k_norm_b"])
    wuq = np.asarray(inp["sa_w_uq"])
    sh["wuqT"] = f(wuq.reshape(NL, 256, 8, 64).transpose(0, 3, 2, 1).reshape(NL, 64, 8 * 256))
    wuk = np.asarray(inp["sa_w_uk"])
    sh["wuk"] = f(wuk.transpose(0, 2, 1, 3).reshape(NL, 64, 8 * 128))
    wuv = np.asarray(inp["sa_w_uv"])
    sh["wuvT"] = f(wuv.transpose(0, 3, 1, 2).reshape(NL, 64, 8 * 128))
    sh["wqidx"] = f(inp["idx_w_q"])
    sh["w_o"] = f(inp["w_o"])
    sh["ln1g"] = rep(inp["ln1_g"])
    sh["ln1b"] = rep(inp["ln1_b"])
    sh["ln2g"] = rep(inp["ln2_g"])
    sh["ln2b"] = rep(inp["ln2_b"])
    sh["router_w"] = f(inp["router_w"])
    sh["router_b"] = rep(inp["router_b"])
    sh["wgu"] = f(inp["w_gate_up"])
    bg = np.asarray(inp["b_gate_up"])
    sh["bgu"] = f(bg.reshape(NL, NE, 16, 128).transpose(0, 3, 1, 2).reshape(NL, 128, NE * 16))
    sh["wdn"] = f(inp["w_down"])
    sh["bdn"] = f(inp["b_down"])
    return sh


_cache = {}


def kernel(**inputs):
    n = 8
    x = np.asarray(inputs["x"], dtype=np.float32)
    shared = prep_inputs(inputs)
    if "nc" not in _cache:
        _cache["nc"] = build()[0]
    nc = _cache["nc"]
    in_maps = []
    for c in range(n):
        m = dict(shared)
        m["x"] = np.ascontiguousarray(x[c])
        in_maps.append(m)
    res = run_bass_kernel_spmd(nc, in_maps, core_ids=list(range(n)))
    return np.stack([np.asarray(r["out"], dtype=np.float32) for r in res.results], axis=0)
```

```python
import numpy as np
import concourse.bass as bass
import concourse.mybir as mybir
from concourse.bass_utils import run_bass_kernel_spmd
from contextlib import ExitStack

F32 = mybir.dt.float32
BF16 = mybir.dt.bfloat16
AF = mybir.ActivationFunctionType
ALU = mybir.AluOpType
AX = mybir.AxisListType

T = 2048
D = 1024
NT = 16
NL = 4
NE = 32
ALPHA = float(8 ** 0.25)
EPS = 1e-6
NBIS = 20
DBG = dict(tiles=NT, stage=99)
TOPK = 256


class Sched:
    ENG = ("pe", "act", "dve", "pool", "sp")

    def __init__(self, nc, es, plan=None):
        self.nc = nc
        self.es = es
        self.plan = plan
        self.dry = plan is None
        self.needed = {n: set() for n in self.ENG}
        self.E = {}
        engs = dict(pe=nc.tensor, act=nc.scalar, dve=nc.vector, pool=nc.gpsimd, sp=nc.sync)
        for name in self.ENG:
            sem = None if self.dry else es.enter_context(nc.semaphore(name + "_sem"))
            self.E[name] = dict(eng=engs[name], sem=sem, count=0, seen={}, name=name)
        if not self.dry:
            self.rank = {}
            for n in self.ENG:
                vals = sorted(plan[n])
                self.rank[n] = {v: i + 1 for i, v in enumerate(vals)}
        self.res = {}
        self.dsem = {}
        self.nwaits = 0
        self.ninst = 0

    def _r(self, key):
        r = self.res.get(key)
        if r is None:
            r = self.res[key] = dict(w=None, r={})
        return r

    def _dma_sem(self, key):
        d = self.dsem.get(key)
        if d is None:
            sem = None if self.dry else self.es.enter_context(self.nc.semaphore("d_%d" % len(self.dsem)))
            d = self.dsem[key] = [sem, 0, "dma:" + str(key)]
        return d

    def _collect(self, ename, reads, writes):
        need = {}

        def add(dep, kind):
            if dep is None:
                return
            sid, val, src = dep
            if src == ename:
                if ename in ("pe", "sp") or kind == "war":
                    return
            cur = need.get(sid)
            if cur is None or cur < val:
                need[sid] = val

        for k in reads:
            add(self._r(k)["w"], "raw")
        for k in writes:
            r = self._r(k)
            add(r["w"], "waw")
            for dep in r["r"].values():
                add(dep, "war")
        return need

    def _emit_waits(self, ename, need):
        E = self.E[ename]
        for sid, val in need.items():
            if E["seen"].get(sid, 0) >= val:
                continue
            E["seen"][sid] = val
            self.nwaits += 1
            if sid in self.E:
                if self.dry:
                    self.needed[sid].add(val)
                else:
                    E["eng"].wait_ge(self.E[sid]["sem"], self.rank[sid][val])
            else:
                if not self.dry:
                    E["eng"].wait_ge(self._dsem_by_id[sid], val)

    def op(self, ename, fn, reads=(), writes=()):
        E = self.E[ename]
        self._emit_waits(ename, self._collect(ename, reads, writes))
        E["count"] += 1
        ins = None
        if not self.dry:
            ins = fn(E["eng"])
            if E["count"] in self.rank[ename]:
                ins.then_inc(E["sem"], 1)
        dep = (ename, E["count"], ename)
        for k in reads:
            self._r(k)["r"][dep[0]] = dep
        for k in writes:
            r = self._r(k)
            r["w"] = dep
            r["r"] = {}
        self.ninst += 1
        return ins

    def dma(self, qname, out, in_, reads=(), writes=(), semkey=None, **kw):
        E = self.E[qname]
        self._emit_waits(qname, self._collect("dma", reads, writes))
        d = self._dma_sem(semkey if semkey is not None else writes[0])
        d[1] += 16
        if not self.dry:
            if not hasattr(self, "_dsem_by_id"):
                self._dsem_by_id = {}
            self._dsem_by_id[d[2]] = d[0]
            ins = E["eng"].dma_start(out=out, in_=in_, **kw)
            ins.then_inc(d[0], 16)
        dep = (d[2], d[1], "dma")
        for k in reads:
            self._r(k)["r"][dep[0]] = dep
        for k in writes:
            r = self._r(k)
            r["w"] = dep
            r["r"] = {}
        self.ninst += 1

    def barrier(self):
        need = {}
        for n, E in self.E.items():
            if E["count"]:
                need[n] = E["count"]
        for d in self.dsem.values():
            if d[1]:
                need[d[2]] = d[1]
        for name in self.E:
            self._emit_waits(name, need)
        self.res = {}

    def mm(self, out, lhsT, rhs, start, stop, r, w):
        return self.op("pe", lambda e: e.matmul(out, lhsT, rhs, start=start, stop=stop), r, w)

    def tr(self, out, in_, ident, r, w):
        return self.op("pe", lambda e: e.transpose(out, in_, ident), list(r) + ["const"], w)

    def act(self, out, in_, func, r, w, **kw):
        return self.op("act", lambda e: e.activation(out, in_, func, **kw), r, w)

    def ts(self, eng, out, in0, s1, s2, op0, op1, r, w, **kw):
        if s2 is None:
            return self.op(eng, lambda e: e.tensor_scalar(out, in0, s1, None, op0, **kw), r, w)
        return self.op(eng, lambda e: e.tensor_scalar(out, in0, s1, s2, op0, op1, **kw), r, w)

    def tt(self, eng, out, in0, in1, op, r, w):
        return self.op(eng, lambda e: e.tensor_tensor(out, in0, in1, op), r, w)

    def stt(self, eng, out, in0, sc, in1, op0, op1, r, w):
        return self.op(eng, lambda e: e.scalar_tensor_tensor(out, in0, sc, in1, op0, op1), r, w)

    def cp(self, eng, out, in_, r, w):
        if eng == "act":
            return self.op("act", lambda e: e.copy(out, in_), r, w)
        return self.op(eng, lambda e: e.tensor_copy(out, in_), r, w)


class Ctx:
    pass


_uid = [0]


def uid(n):
    _uid[0] += 1
    return "%s_%d" % (n, _uid[0])


def rstd_from_ss(S, A, ss, n, tag, rk):
    S.act(ss, ss, AF.Sqrt, [rk], [rk], scale=1.0 / n, bias=A.eps[:, 0:1])
    S.op("dve", lambda e: e.reciprocal(ss, ss), [rk], [rk])


def setup_consts(C, es):
    nc, S = C.nc, C.S
    A = C
    A.ident = es.enter_context(nc.sbuf_tensor("ident", [128, 128], F32))
    A.identb = es.enter_context(nc.sbuf_tensor("identb", [128, 128], BF16))
    A.ones = es.enter_context(nc.sbuf_tensor("ones", [128, 128], F32))
    A.onesb = es.enter_context(nc.sbuf_tensor("onesb", [128, 128], BF16))
    A.tril = es.enter_context(nc.sbuf_tensor("tril", [128, 128], F32))
    A.trils = es.enter_context(nc.sbuf_tensor("trils", [128, 128], F32))
    A.triu = es.enter_context(nc.sbuf_tensor("triu", [128, 128], F32))
    A.cmask = es.enter_context(nc.sbuf_tensor("cmask", [128, 128], F32))
    A.eps = es.enter_context(nc.sbuf_tensor("epsc", [128, 1], F32))
    g = nc.gpsimd
    k = ["const"]
    S.op("pool", lambda e: e.memset(A.ident[:], 0.0), [], k)
    S.op("pool", lambda e: e.affine_select(out=A.ident[:], in_=A.ident[:], pattern=[[-1, 128]],
                                           compare_op=ALU.not_equal, fill=1.0, base=0, channel_multiplier=1), k, k)
    S.op("pool", lambda e: e.memset(A.ones[:], 1.0), [], k)
    S.op("pool", lambda e: e.memset(A.eps[:], EPS), [], k)
    S.op("pool", lambda e: e.affine_select(out=A.tril[:], in_=A.ones[:], pattern=[[-1, 128]],
                                           compare_op=ALU.is_ge, fill=0.0, base=0, channel_multiplier=1), k, k)
    S.op("pool", lambda e: e.affine_select(out=A.trils[:], in_=A.ones[:], pattern=[[-1, 128]],
                                           compare_op=ALU.is_gt, fill=0.0, base=0, channel_multiplier=1), k, k)
    S.op("pool", lambda e: e.affine_select(out=A.triu[:], in_=A.ones[:], pattern=[[1, 128]],
                                           compare_op=ALU.is_ge, fill=0.0, base=0, channel_multiplier=-1), k, k)
    S.ts("pool", A.cmask[:], A.tril[:], -1.0, 1e30, ALU.add, ALU.mult, k, k)
    S.cp("pool", A.identb[:], A.ident[:], k, k)
    S.cp("pool", A.onesb[:], A.ones[:], k, k)


def phase_dsa(C, l):
    nc, S = C.nc, C.S
    xT, olT = C.xT, C.olT
    with ExitStack() as es:
        A = lambda n, sh, dt: es.enter_context(nc.sbuf_tensor(uid(n), sh, dt))
        P = lambda n, sh, dt: es.enter_context(nc.psum_tensor(uid(n), sh, dt))
        w_dsa = A("w_dsa", [128, 8, 456], BF16)
        wqidx = A("wqidx", [128, 2, 512], F32)
        wuqT = A("wuqT", [64, 8, 256], F32)
        wuk = A("wuk", [64, 8, 128], F32)
        qn = A("qn", [128, 256], F32)
        kvn = A("kvn", [128, 128], F32)
        ig = A("ig", [128, 64], F32)
        ib = A("ib", [128, 64], F32)
        Wql = A("Wql", [128, 2, 8, 128], F32)
        kT2 = A("kT2", [128, T], F32)
        ckvT = A("ckvT", [128, T], BF16)
        ckvt = A("ckvt", [128, NT, 128], BF16)
        pj = [A("pj", [128, 456], F32) for _ in range(2)]
        cqn = [A("cqn", [128, 256], F32) for _ in range(2)]
        kdup = [A("kdup", [128, 128], F32) for _ in range(2)]
        st = [A("st", [128, 16], F32) for _ in range(2)]
        wsm = [A("wsm", [128, 16], F32) for _ in range(2)]
        cqnT = [A("cqnT", [128, 2, 128], F32) for _ in range(2)]
        qiT = [A("qiT", [128, 4, 128], F32) for _ in range(2)]
        qlT = [A("qlT", [128, 8, 128], BF16) for _ in range(2)]
        score = [A("score", [128, T], F32)] * 2
        Rt = [A("Rt", [128, 512], F32) for _ in range(2)]
        junk = A("junk", [128, T], BF16)
        junkf = A("junkf", [128, 256], F32)
        Mk = [A("Mk", [128, T], BF16)] * 2
        MT = [A("MT", [128, NT, 128], BF16) for _ in range(2)]
        pT = [A("pT", [128, 512], BF16) for _ in range(3)]
        bs = [A("bs", [128, 8], F32) for _ in range(2)]
        rinv = A("rinv", [128, 512], F32)
        pa = P("pa", [128, 512], F32)
        pb = P("pb", [128, 512], F32)
        pX = [P("pX", [128, 512], F32) for _ in range(2)]
        pMT = P("pMT", [128, 1024], BF16)
        pO = P("pO", [128, 512], F32)
        pR = P("pR", [128, 512], F32)

        S.dma("pool", w_dsa[:], C.w_in[l].rearrange("(c p) n -> p c n", p=128)[:, :, 2056:2512], [], ["w_dsa"])
        S.dma("sp", wqidx[:], C.wqidx[l].rearrange("(c p) n -> p c n", p=128), [], ["wqidx"])
        S.dma("sp", wuqT[:], C.wuqT[l].rearrange("p (h c) -> p h c", h=8), [], ["wuqT"])
        S.dma("sp", wuk[:], C.wuk[l].rearrange("p (h r) -> p h r", h=8), [], ["wuk"])
        S.dma("sp", qn[:], C.qn[l], [], ["qn"])
        S.dma("sp", kvn[:], C.kvn[l], [], ["kvn"])
        S.dma("sp", ig[:], C.ig[l], [], ["ig"])
        S.dma("sp", ib[:], C.ib[l], [], ["ib"])
        for c in range(2):
            for hg in range(2):
                pk = "pa" if hg == 0 else "pb"
                pp = pa if hg == 0 else pb
                for h4 in range(4):
                    h = hg * 4 + h4
                    S.mm(pp[:, h4 * 128:(h4 + 1) * 128], wuqT[:, h, c * 128:(c + 1) * 128], wuk[:, h, :],
                         True, True, ["wuqT", "wuk"], [pk])
                S.act(Wql[:, c, hg * 4:(hg + 1) * 4, :], pp[:].rearrange("p (h r) -> p h r", h=4), AF.Copy,
                      [pk], ["Wql"], scale=0.125)

        for i in range(DBG["tiles"]):
            b = i % 2
            nk = 128 * (i + 1)
            ts_ = slice(i * 128, (i + 1) * 128)
            for dc in range(8):
                S.mm(pa[:, 0:456], xT[:, dc, ts_], w_dsa[:, dc, :], dc == 0, dc == 7, ["xT", "w_dsa"], ["pa"])
            kpj = ("pj", b)
            S.cp("act", pj[b][:], pa[:, 0:456], ["pa"], [kpj])
            cq = pj[b][:, 0:256]
            ckv = pj[b][:, 256:384]
            ik = pj[b][:, 384:448]
            iw = pj[b][:, 448:456]
            kst = ("st", b)
            S.act(junkf[:, 0:256], cq, AF.Square, [kpj], ["junkf", kst], accum_out=st[b][:, 0:1])
            S.act(junkf[:, 0:128], ckv, AF.Square, [kpj], ["junkf", kst], accum_out=st[b][:, 1:2])
            S.act(junkf[:, 0:64], ik, AF.Identity, [kpj], ["junkf", kst], accum_out=st[b][:, 2:3], scale=1.0 / 64)
            S.act(st[b][:, 0:1], st[b][:, 0:1], AF.Sqrt, [kst], [kst], scale=1.0 / 256, bias=C.eps[:, 0:1])
            S.act(st[b][:, 1:2], st[b][:, 1:2], AF.Sqrt, [kst], [kst], scale=1.0 / 128, bias=C.eps[:, 0:1])
            S.op("dve", lambda e: e.reciprocal(st[b][:, 0:2], st[b][:, 0:2]), [kst], [kst])
            kcqn = ("cqn", b)
            S.stt("dve", cqn[b][:], cq, st[b][:, 0:1], qn[:], ALU.mult, ALU.mult, [kpj, kst, "qn"], [kcqn])
            S.stt("dve", ckvt[:, i, :], ckv, st[b][:, 1:2], kvn[:], ALU.mult, ALU.mult, [kpj, kst, "kvn"], ["ckvt"])
            kkd = ("kdup", b)
            S.ts("dve", kdup[b][:, 0:64], ik, st[b][:, 2:3], None, ALU.subtract, None, [kpj, kst], [kkd])
            S.act(junkf[:, 0:64], kdup[b][:, 0:64], AF.Square, [kkd], ["junkf", kst], accum_out=st[b][:, 3:4])
            S.act(st[b][:, 3:4], st[b][:, 3:4], AF.Sqrt, [kst], [kst], scale=1.0 / 64, bias=C.eps[:, 0:1])
            S.op("dve", lambda e: e.reciprocal(st[b][:, 3:4], st[b][:, 3:4]), [kst], [kst])
            S.stt("dve", kdup[b][:, 0:64], kdup[b][:, 0:64], st[b][:, 3:4], ig[:], ALU.mult, ALU.mult,
                  [kkd, kst, "ig"], [kkd])
            S.tt("dve", kdup[b][:, 64:128], kdup[b][:, 0:64], ib[:], ALU.add, [kkd, "ib"], [kkd])
            S.tt("dve", kdup[b][:, 0:64], kdup[b][:, 0:64], ib[:], ALU.add, [kkd, "ib"], [kkd])
            kws = ("wsm", b)
            S.act(wsm[b][:, 0:8], iw, AF.Abs, [kpj], [kws], scale=float(8 ** -0.5 * 64 ** -0.5))
            S.act(wsm[b][:, 8:16], iw, AF.Sign, [kpj], [kws])
            if DBG["stage"] < 2:
                continue
            sub = DBG.get("sub", 0)
            if sub in (0, 1, 5, 6, 7, 8):
                S.tr(pb[:, 0:128], cqn[b][:, 0:128], C.ident[:], [kcqn], ["pb"])
            if sub in (0, 1, 5, 7, 8):
                S.tr(pb[:, 128:256], cqn[b][:, 128:256], C.ident[:], [kcqn], ["pb"])
                S.tr(pb[:, 256:384], kdup[b][:], C.ident[:], [kkd], ["pb"])
            kcT = ("cqnT", b)
            if sub in (0, 1, 3, 7):
                S.cp("act", cqnT[b][:], pb[:, 0:256].rearrange("p (c q) -> p c q", c=2), ["pb"], [kcT])
            if sub in (0, 1, 4, 8):
                S.cp("act", kT2[:, ts_], pb[:, 256:384], ["pb"], ["kT2"])
            if sub in (0, 2):
                S.tr(pMT[:, 0:128], ckvt[:, i, :], C.identb[:], ["ckvt"], ["pMT"])
                S.cp("act", ckvT[:, ts_], pMT[:, 0:128], ["pMT"], ["ckvT"])
            if DBG["stage"] < 3:
                continue
            for pr in range(4):
                for c in range(2):
                    S.mm(pa[:, pr * 128:(pr + 1) * 128], wqidx[:, c, pr * 128:(pr + 1) * 128], cqnT[b][:, c, :],
                         c == 0, c == 1, ["wqidx", kcT], ["pa"])
            kqi = ("qiT", b)
            S.cp("act", qiT[b][:], pa[:].rearrange("p (h q) -> p h q", h=4), ["pa"], [kqi])
            kql = ("qlT", b)
            for hg in range(2):
                for h4 in range(4):
                    for c in range(2):
                        S.mm(pb[:, h4 * 128:(h4 + 1) * 128], Wql[:, c, hg * 4 + h4, :], cqnT[b][:, c, :],
                             c == 0, c == 1, ["Wql", kcT], ["pb"])
                S.cp("dve", qlT[b][:, hg * 4:(hg + 1) * 4, :], pb[:].rearrange("p (h q) -> p h q", h=4), ["pb"], [kql])
            if DBG["stage"] < 4:
                continue
            ksc = "score"
            nsg = (nk + 511) // 512
            cnt = 0
            for sg in range(nsg):
                w = min(512, nk - sg * 512)
                cs = slice(sg * 512, sg * 512 + w)
                for h in range(8):
                    px = pX[cnt % 2]
                    kpx = ("pX", cnt % 2)
                    kr = ("Rt", cnt % 2)
                    pr0 = (h % 2) * 64
                    S.mm(px[:, 0:w], qiT[b][pr0:pr0 + 64, h // 2, :], kT2[pr0:pr0 + 64, cs], True, True,
                         [kqi, "kT2"], [kpx])
                    S.act(Rt[cnt % 2][:, 0:w], px[:, 0:w], AF.Relu, [kpx, kws], [kr], scale=wsm[b][:, h:h + 1])
                    if h == 0:
                        S.ts("dve", score[b][:, cs], Rt[cnt % 2][:, 0:w], wsm[b][:, 8:9], None, ALU.mult, None,
                             [kr, kws], [ksc])
                    else:
                        S.stt("dve", score[b][:, cs], Rt[cnt % 2][:, 0:w], wsm[b][:, 8 + h:9 + h], score[b][:, cs],
                              ALU.mult, ALU.add, [kr, kws, ksc], [ksc])
                    cnt += 1
            kbs = ("bs", b)
            B = bs[b]
            S.op("dve", lambda e: e.tensor_reduce(B[:, 5:6], score[b][:, 0:nk], AX.X, ALU.max,
                                                  apply_absolute_value=True), [ksc], [kbs])
            S.tt("dve", score[b][:, nk - 128:nk], score[b][:, nk - 128:nk], C.cmask[:], ALU.add, [ksc, "const"], [ksc])
            S.ts("dve", B[:, 0:1], B[:, 5:6], 1.0, -1.0, ALU.add, ALU.mult, [kbs], [kbs])
            S.ts("dve", B[:, 1:2], B[:, 5:6], 1.0, 2.0, ALU.add, ALU.mult, [kbs], [kbs])

            def gen_B(i=i, b=b, nk=nk, B=B, kbs=kbs, ksc=ksc):
                for it in range(NBIS):
                    cst = float(2.0 ** -(it + 1))
                    S.stt("dve", B[:, 2:3], B[:, 1:2], cst, B[:, 0:1], ALU.mult, ALU.add, [kbs], [kbs])
                    S.ts("dve", junk[:, 0:nk], score[b][:, 0:nk], B[:, 2:3], 0.0, ALU.is_ge, ALU.add, [ksc, kbs],
                         ["junk", kbs], accum_out=B[:, 3:4])
                    S.ts("dve", B[:, 4:5], B[:, 3:4], float(TOPK), cst, ALU.is_ge, ALU.mult, [kbs], [kbs])
                    S.stt("dve", B[:, 0:1], B[:, 4:5], B[:, 1:2], B[:, 0:1], ALU.mult, ALU.add, [kbs], [kbs])
                    yield

            def gen_C(i, b):
                ts_c = slice(i * 128, (i + 1) * 128)
                kql_, kmt_ = ("qlT", b), ("MT", b)
                stp = [(hg, j) for hg in range(2) for j in range(i + 1)]

                def smm(k):
                    hg, j = stp[k]
                    qv = qlT[b][:, hg * 4:(hg + 1) * 4, :].rearrange("p h q -> p (h q)")
                    S.mm(pX[k % 2][:, :], ckvT[:, j * 128:(j + 1) * 128], qv, True, True, ["ckvT", kql_],
                         [("pX", k % 2)])

                smm(0)
                for k, (hg, j) in enumerate(stp):
                    if k + 1 < len(stp):
                        smm(k + 1)
                    px, kpx = pX[k % 2], ("pX", k % 2)
                    pt, kpt = pT[k % 3], ("pT", k % 3)
                    S.act(pt[:], px[:], AF.Exp, [kpx], [kpt])
                    S.tt("dve", pt[:].rearrange("p (h q) -> p h q", h=4), pt[:].rearrange("p (h q) -> p h q", h=4),
                         MT[b][:, j:j + 1, :].to_broadcast([128, 4, 128]), ALU.mult, [kpt, kmt_], [kpt])
                    S.mm(pO[:], ckvt[:, j, :], pt[:], j == 0, j == i, ["ckvt", kpt], ["pO"])
                    S.mm(pR[:], C.onesb[:], pt[:], j == 0, j == i, ["const", kpt], ["pR"])
                    if j == i:
                        S.act(rinv[:], pR[:], AF.Ln, ["pR"], ["rinv"])
                        S.act(rinv[:], rinv[:], AF.Exp, ["rinv"], ["rinv"], scale=-1.0)
                        S.tt("dve", olT[:, hg * 4:(hg + 1) * 4, ts_c], pO[:].rearrange("p (h q) -> p h q", h=4),
                             rinv[:].rearrange("p (h q) -> p h q", h=4), ALU.mult, ["pO", "rinv"], ["olT"])
                    yield

            gens = [gen_B()]
            if i > 0:
                gens.append(gen_C(i - 1, 1 - b))
            while gens:
                for g_ in list(gens):
                    try:
                        next(g_)
                    except StopIteration:
                        gens.remove(g_)
            kmk = "Mk"
            S.ts("dve", Mk[b][:, 0:nk], score[b][:, 0:nk], B[:, 0:1], None, ALU.is_ge, None, [ksc, kbs], [kmk])
            kmt = ("MT", b)
            for j0 in range(0, i + 1, 8):
                nj = min(8, i + 1 - j0)
                for j in range(j0, j0 + nj):
                    S.tr(pMT[:, (j - j0) * 128:(j - j0 + 1) * 128], Mk[b][:, j * 128:(j + 1) * 128], C.identb[:],
                         [kmk], ["pMT"])
                S.cp("act", MT[b][:, j0:j0 + nj, :], pMT[:, 0:nj * 128].rearrange("p (j q) -> p j q", j=nj),
                     ["pMT"], [kmt])
            if i == NT - 1:
                for _ in gen_C(i, b):
                    pass
        S.barrier()


def phase_dn(C, l):
    nc, S = C.nc, C.S
    xT, ydT = C.xT, C.ydT
    with ExitStack() as es:
        A = lambda n, sh, dt: es.enter_context(nc.sbuf_tensor(uid(n), sh, dt))
        P = lambda n, sh, dt: es.enter_context(nc.psum_tensor(uid(n), sh, dt))
        w_f = A("w_f", [128, 8, 1536], BF16)
        w_t = A("w_t", [128, 8, 520], BF16)
        convw = A("convw", [128, 48], F32)
        alog = A("alog", [128, 4], F32)
        dtb = A("dtb", [128, 4], F32)
        nrm = A("nrm", [128, 128], F32)
        Sst = A("Sst", [128, 4, 128], F32)
        xs = [A("xs", [128, 12, 131], F32) for _ in range(2)]
        cv = [A("cv", [128, 12, 128], F32)] * 2
        sq = A("sq", [128, 8, 128], F32)
        ctmp = A("ctmp", [128, 6, 128], F32)
        qT = [A("qT", [128, 4, 128], F32)] * 2
        kT = [A("kT", [128, 4, 128], F32)] * 2
        ktok = [A("ktok", [128, 4, 128], F32)] * 2
        vtok = [A("vtok", [128, 4, 128], F32)] * 2
        pjt = [A("pjt", [128, 520], F32)] * 2
        sz = [A("sz", [128, 512], F32)] * 2
        sm = [A("sm", [128, 48], F32)] * 2
        Gb = [A("Gb", [128, 4, 128], F32)] * 2
        Dm = [A("Dm", [128, 4, 128], F32)] * 2
        Ds = [A("Ds", [128, 4, 128], F32)] * 2
        Pm = [A("Pm", [128, 4, 128], F32) for _ in range(2)]
        PmT = [A("PmT", [128, 4, 128], F32) for _ in range(2)]
        Tt = [A("Tt", [128, 4, 128], F32) for _ in range(2)]
        Aq = A("Aq", [128, 4, 128], F32)
        gB = A("gB", [128, 4, 128], F32)
        AqT = A("AqT", [128, 4, 128], F32)
        kbg = A("kbg", [128, 4, 128], F32)
        kd = A("kd", [128, 4, 128], F32)
        vb = A("vb", [128, 4, 128], F32)
        wT = A("wT", [128, 4, 128], F32)
        vn = A("vn", [128, 4, 128], F32)
        t1 = A("t1", [128, 4, 128], F32)
        oo = A("oo", [128, 4, 128], F32)
        yd = A("yd", [128, 512], BF16)
        p0 = P("p0", [128, 512], F32)
        p1 = P("p1", [128, 512], F32)
        p2 = P("p2", [128, 512], F32)
        p3 = P("p3", [128, 512], F32)
        p4 = P("p4", [128, 512], F32)
        p5 = P("p5", [128, 512], F32)
        pTb = P("pTb", [128, 1024], BF16)

        wv = C.w_in[l].rearrange("(c p) n -> p c n", p=128)
        S.dma("pool", w_f[:, :, 0:768], wv[:, :, 0:768], [], ["w_f"])
        S.dma("pool", w_f[:, :, 768:1536], wv[:, :, 768:1536], [], ["w_f"])
        S.dma("pool", w_t[:], wv[:, :, 1536:2056], [], ["w_t"])
        S.dma("sp", convw[:], C.convw[l], [], ["convw"])
        S.dma("sp", alog[:], C.alog[l], [], ["alog"])
        S.dma("sp", dtb[:], C.dtb[l], [], ["dtb"])
        S.dma("sp", nrm[:], C.dnnorm[l], [], ["nrm"])
        S.op("dve", lambda e: e.memset(Sst[:], 0.0), [], ["Sst"])
        S.op("dve", lambda e: e.memset(xs[1][:, :, 128:131], 0.0), [], [("xs", 1)])
        S.act(alog[:], alog[:], AF.Exp, ["alog"], ["alog"])
        S.ts("dve", alog[:], alog[:], -1.0, None, ALU.mult, None, ["alog"], ["alog"])

        def h4(t):
            return t[:].rearrange("p h c -> p (h c)")

        for i in range(NT):
            b = i % 2
            ts_ = slice(i * 128, (i + 1) * 128)
            kxs, kxp = ("xs", b), ("xs", 1 - b)
            S.cp("dve", xs[b][:, :, 0:3], xs[1 - b][:, :, 128:131], [kxp], [kxs])
            for g3 in range(3):
                pp, pk = [(p0, "p0"), (p1, "p1"), (p2, "p2")][g3]
                for c4 in range(4):
                    ct = g3 * 4 + c4
                    for dc in range(8):
                        S.mm(pp[:, c4 * 128:(c4 + 1) * 128], w_f[:, dc, ct * 128:(ct + 1) * 128], xT[:, dc, ts_],
                             dc == 0, dc == 7, ["w_f", "xT"], [pk])
                S.cp("act", xs[b][:, g3 * 4:(g3 + 1) * 4, 3:131], pp[:].rearrange("p (c t) -> p c t", c=4), [pk], [kxs])
            for (c0, c1, pp, pk) in [(0, 512, p3, "p3"), (512, 520, p4, "p4")]:
                for dc in range(8):
                    S.mm(pp[:, 0:c1 - c0], xT[:, dc, ts_], w_t[:, dc, c0:c1], dc == 0, dc == 7, ["xT", "w_t"], [pk])
            kpj = "pjt_"
            ksz = "sz_"
            S.act(sz[b][:], p3[:, 0:512], AF.Silu, ["p3"], [ksz])
            S.cp("dve", pjt[b][:, 512:520], p4[:, 0:8], ["p4"], [kpj])
            kcv = "cv_"
            cwv = convw[:].rearrange("p (c k) -> p c k", k=4)
            for h6 in range(2):
                cs6 = slice(h6 * 6, (h6 + 1) * 6)
                S.tt("dve", cv[b][:, cs6, :], xs[b][:, cs6, 3:131], cwv[:, cs6, 3:4].to_broadcast([128, 6, 128]),
                     ALU.mult, [kxs, "convw"], [kcv])
                for k in range(3):
                    S.tt("dve", ctmp[:], xs[b][:, cs6, k:k + 128], cwv[:, cs6, k:k + 1].to_broadcast([128, 6, 128]),
                         ALU.mult, [kxs, "convw"], ["ctmp"])
                    S.tt("dve", cv[b][:, cs6, :], cv[b][:, cs6, :], ctmp[:], ALU.add, [kcv, "ctmp"], [kcv])
            S.act(cv[b][:], cv[b][:], AF.Silu, [kcv], [kcv])
            S.act(sq[:], cv[b][:, 0:8, :], AF.Square, [kcv], ["sq"])
            for hh in range(2):
                pp, pk = [(p0, "p0"), (p1, "p1")][hh]
                S.mm(pp[:], C.ones[:], sq[:, hh * 4:(hh + 1) * 4, :].rearrange("p h c -> p (h c)"), True, True,
                     ["const", "sq"], [pk])
                S.act(sq[:, hh * 4:(hh + 1) * 4, :].rearrange("p h c -> p (h c)"), pp[:], AF.Ln, [pk], ["sq"],
                      bias=C.eps[:, 0:1])
            S.act(sq[:], sq[:], AF.Exp, ["sq"], ["sq"], scale=-0.5)
            kq, kk = "qT_", "kT_"
            S.stt("dve", qT[b][:], cv[b][:, 0:4, :], float(128 ** -0.5), sq[:, 0:4, :], ALU.mult, ALU.mult,
                  [kcv, "sq"], [kq])
            S.tt("dve", kT[b][:], cv[b][:, 4:8, :], sq[:, 4:8, :], ALU.mult, [kcv, "sq"], [kk])
            for hh in range(4):
                S.tr(p0[:, hh * 128:(hh + 1) * 128], kT[b][:, hh, :], C.ident[:], [kk], ["p0"])
                S.tr(p1[:, hh * 128:(hh + 1) * 128], cv[b][:, 8 + hh, :], C.ident[:], [kcv], ["p1"])
            kkt, kvt = "ktok_", "vtok_"
            S.cp("act", h4(ktok[b]), p0[:], ["p0"], [kkt])
            S.cp("act", h4(vtok[b]), p1[:], ["p1"], [kvt])
            ksm = "sm_"
            M_ = sm[b]
            S.act(M_[:, 0:4], pjt[b][:, 512:516], AF.Sigmoid, [kpj], [ksm])
            S.tt("dve", M_[:, 24:28], pjt[b][:, 516:520], dtb[:], ALU.add, [kpj, "dtb"], [ksm])
            S.act(M_[:, 28:32], M_[:, 24:28], AF.Abs, [ksm], [ksm])
            S.act(M_[:, 28:32], M_[:, 28:32], AF.Exp, [ksm], [ksm], scale=-1.0)
            S.act(M_[:, 28:32], M_[:, 28:32], AF.Ln, [ksm], [ksm], bias=C.ones[:, 0:1])
            S.stt("dve", M_[:, 24:28], M_[:, 24:28], 0.0, M_[:, 28:32], ALU.max, ALU.add, [ksm], [ksm])
            S.tt("dve", M_[:, 4:8], M_[:, 24:28], alog[:], ALU.mult, [ksm, "alog"], [ksm])
            S.mm(p2[:, 0:4], C.triu[:], M_[:, 4:8], True, True, ["const", ksm], ["p2"])
            kgb = "Gb_"
            S.cp("dve", gB[:], M_[:, 4:8].unsqueeze(2).to_broadcast([128, 4, 128]), [ksm], ["gB"])
            for hh in range(4):
                S.mm(p3[:, hh * 128:(hh + 1) * 128], gB[:, hh, :], C.triu[:], True, True, ["gB", "const"], ["p3"])
            S.cp("dve", M_[:, 8:12], p2[:, 0:4], ["p2"], [ksm])
            S.cp("act", h4(Gb[b]), p3[:], ["p3"], [kgb])
            S.act(M_[:, 12:16], M_[:, 8:12], AF.Exp, [ksm], [ksm])
            S.tt("dve", M_[:, 16:20], M_[:, 12:16], M_[:, 0:4], ALU.mult, [ksm], [ksm])
            S.tt("dve", M_[:, 20:24], Gb[b][:, :, 127], M_[:, 8:12], ALU.subtract, [kgb, ksm], [ksm])
            S.act(M_[:, 20:24], M_[:, 20:24], AF.Exp, [ksm], [ksm])
            S.act(M_[:, 32:36], Gb[b][:, :, 127], AF.Exp, [kgb], [ksm])
            S.ts("dve", M_[:, 36:40], M_[:, 0:4], -1.0, None, ALU.mult, None, [ksm], [ksm])
            kdm, kds = "Dm_", "Ds_"
            for hh in range(4):
                S.ts("dve", Dm[b][:, hh, :], Gb[b][:, hh, :], M_[:, 8 + hh:9 + hh], 0.0, ALU.subtract, ALU.max,
                     [kgb, ksm], [kdm])
            S.act(h4(Dm[b]), h4(Dm[b]), AF.Exp, [kdm], [kdm], scale=-1.0)
            S.tt("pool", Ds[b][:], Dm[b][:], C.trils[:].unsqueeze(1).to_broadcast([128, 4, 128]), ALU.mult,
                 [kdm, "const"], [kds])
            S.tt("pool", Dm[b][:], Dm[b][:], C.tril[:].unsqueeze(1).to_broadcast([128, 4, 128]), ALU.mult,
                 [kdm, "const"], [kdm])
            for hh in range(4):
                S.mm(p0[:, hh * 128:(hh + 1) * 128], kT[b][:, hh, :], kT[b][:, hh, :], True, True, [kk], ["p0"])
                S.mm(p1[:, hh * 128:(hh + 1) * 128], qT[b][:, hh, :], kT[b][:, hh, :], True, True, [kq, kk], ["p1"])
            kpm, kpmt, ktt = ("Pm", 0), ("PmT", 0), ("Tt", 0)
            for hh in range(4):
                S.stt("dve", Pm[0][:, hh, :], p0[:, hh * 128:(hh + 1) * 128], M_[:, 36 + hh:37 + hh], Ds[b][:, hh, :],
                      ALU.mult, ALU.mult, ["p0", ksm, kds], [kpm])
            S.tt("dve", h4(Aq), p1[:], h4(Dm[b]), ALU.mult, ["p1", kdm], ["Aq"])
            for hh in range(4):
                S.tr(p2[:, hh * 128:(hh + 1) * 128], Pm[0][:, hh, :], C.ident[:], [kpm], ["p2"])
                S.tr(p3[:, hh * 128:(hh + 1) * 128], Aq[:, hh, :], C.ident[:], ["Aq"], ["p3"])
            S.cp("act", h4(PmT[0]), p2[:], ["p2"], [kpmt])
            S.cp("act", h4(AqT), p3[:], ["p3"], ["AqT"])
            S.tt("dve", Tt[0][:], PmT[0][:], C.ident[:].unsqueeze(1).to_broadcast([128, 4, 128]), ALU.add,
                 [kpmt, "const"], [ktt])
            cur = 0
            for lev in range(1, 7):
                nxt = 1 - cur
                kpm_n, kpmt_n, ktt_n = ("Pm", nxt), ("PmT", nxt), ("Tt", nxt)
                kpm_c, kpmt_c, ktt_c = ("Pm", cur), ("PmT", cur), ("Tt", cur)
                for hh in range(4):
                    S.mm(p0[:, hh * 128:(hh + 1) * 128], PmT[cur][:, hh, :], Pm[cur][:, hh, :], True, True,
                         [kpm_c, kpmt_c], ["p0"])
                    if lev < 6:
                        S.mm(p1[:, hh * 128:(hh + 1) * 128], Pm[cur][:, hh, :], PmT[cur][:, hh, :], True, True,
                             [kpm_c, kpmt_c], ["p1"])
                S.cp("act", h4(Pm[nxt]), p0[:], ["p0"], [kpm_n])
                if lev < 6:
                    S.cp("dve", h4(PmT[nxt]), p1[:], ["p1"], [kpmt_n])
                for hh in range(4):
                    S.mm(p2[:, hh * 128:(hh + 1) * 128], Pm[nxt][:, hh, :], Tt[cur][:, hh, :], True, True,
                         [kpm_n, ktt_c], ["p2"])
                S.tt("dve", h4(Tt[nxt]), p2[:], h4(Tt[cur]), ALU.add, ["p2", ktt_c], [ktt_n])
                cur = nxt
            ktt = ("Tt", cur)
            TT = Tt[cur]
            for hh in range(4):
                S.ts("pool", kbg[:, hh, :], ktok[b][:, hh, :], M_[:, 16 + hh:17 + hh], None, ALU.mult, None,
                     [kkt, ksm], ["kbg"])
                S.ts("pool", kd[:, hh, :], ktok[b][:, hh, :], M_[:, 20 + hh:21 + hh], None, ALU.mult, None,
                     [kkt, ksm], ["kd"])
                S.ts("pool", vb[:, hh, :], vtok[b][:, hh, :], M_[:, 0 + hh:1 + hh], None, ALU.mult, None,
                     [kvt, ksm], ["vb"])
            for hh in range(4):
                S.mm(p0[:, hh * 128:(hh + 1) * 128], kbg[:, hh, :], TT[:, hh, :], True, True, ["kbg", ktt], ["p0"])
            S.act(h4(wT), p0[:], AF.Copy, ["p0"], ["wT"], scale=-1.0)
            for hh in range(4):
                S.mm(p1[:, hh * 128:(hh + 1) * 128], TT[:, hh, :], vb[:, hh, :], True, False, [ktt, "vb"], ["p1"])
                S.mm(p1[:, hh * 128:(hh + 1) * 128], wT[:, hh, :], Sst[:, hh, :], False, True, ["wT", "Sst"], ["p1"])
            S.cp("act", h4(vn), p1[:], ["p1"], ["vn"])
            for hh in range(4):
                S.mm(p2[:, hh * 128:(hh + 1) * 128], qT[b][:, hh, :], Sst[:, hh, :], True, True, [kq, "Sst"], ["p2"])
                S.mm(p3[:, hh * 128:(hh + 1) * 128], AqT[:, hh, :], vn[:, hh, :], True, True, ["AqT", "vn"], ["p3"])
            for hh in range(4):
                S.act(t1[:, hh, :], p2[:, hh * 128:(hh + 1) * 128], AF.Copy, ["p2", ksm], ["t1"],
                      scale=M_[:, 12 + hh:13 + hh])
            S.tt("dve", h4(oo), p3[:], h4(t1), ALU.add, ["p3", "t1"], ["oo"])
            for hh in range(4):
                S.mm(p4[:, hh * 128:(hh + 1) * 128], kd[:, hh, :], vn[:, hh, :], True, True, ["kd", "vn"], ["p4"])
            for hh in range(4):
                S.stt("dve", Sst[:, hh, :], Sst[:, hh, :], M_[:, 32 + hh:33 + hh], p4[:, hh * 128:(hh + 1) * 128],
                      ALU.mult, ALU.add, ["Sst", ksm, "p4"], ["Sst"])
            for hh in range(4):
                S.act(t1[:, hh, :], oo[:, hh, :], AF.Square, ["oo"], ["t1", ksm], accum_out=M_[:, 40 + hh:41 + hh])
            S.act(M_[:, 40:44], M_[:, 40:44], AF.Sqrt, [ksm], [ksm], scale=1.0 / 128, bias=C.eps[:, 0:1])
            S.op("dve", lambda e: e.reciprocal(M_[:, 40:44], M_[:, 40:44]), [ksm], [ksm])
            for hh in range(4):
                S.stt("dve", oo[:, hh, :], oo[:, hh, :], M_[:, 40 + hh:41 + hh], nrm[:], ALU.mult, ALU.mult,
                      ["oo", ksm, "nrm"], ["oo"])
            S.tt("dve", yd[:], h4(oo), sz[b][:], ALU.mult, ["oo", ksz], ["yd"])
            for hh in range(4):
                S.tr(pTb[:, hh * 128:(hh + 1) * 128], yd[:, hh * 128:(hh + 1) * 128], C.identb[:], ["yd"], ["pTb"])
            S.cp("act", ydT[:, :, ts_], pTb[:, 0:512].rearrange("p (h c) -> p h c", h=4), ["pTb"], ["ydT"])
        S.barrier()


def layer_norm_tile(C, S, src, dst, g_bc, b_bc, st, kst, rsrc, kdst, junk):
    S.act(junk, src, AF.Identity, rsrc, ["lnjunk", kst], accum_out=st[:, 0:1], scale=1.0 / D)
    S.ts("dve", src, src, st[:, 0:1], None, ALU.subtract, None, rsrc + [kst], rsrc)
    S.act(junk, src, AF.Square, rsrc, ["lnjunk", kst], accum_out=st[:, 1:2])
    S.act(st[:, 1:2], st[:, 1:2], AF.Sqrt, [kst], [kst], scale=1.0 / D, bias=C.eps[:, 0:1])
    S.op("dve", lambda e: e.reciprocal(st[:, 1:2], st[:, 1:2]), [kst], [kst])
    S.stt("dve", src, src, st[:, 1:2], g_bc, ALU.mult, ALU.mult, rsrc + [kst, "lng"], rsrc)
    S.tt("pool", dst, src, b_bc, ALU.add, rsrc + ["lnb"], [kdst])


def phase_c(C, l, x_src, x_dst):
    nc, S = C.nc, C.S
    with ExitStack() as es:
        A = lambda n, sh, dt: es.enter_context(nc.sbuf_tensor(uid(n), sh, dt))
        P = lambda n, sh, dt: es.enter_context(nc.psum_tensor(uid(n), sh, dt))
        wod = A("wod", [128, 4, D], BF16)
        wos = A("wos", [64, 8, D], F32)
        wuvT = A("wuvT", [64, 8, 128], F32)
        Wvo = A("Wvo", [128, 8, D], BF16)
        lng = A("lng", [128, D], F32)
        lnb = A("lnb", [128, D], F32)
        rw = A("rw", [128, 8, NE], F32)
        rb = A("rb", [128, NE], F32)
        xin = [A("xin", [128, D], F32) for _ in range(2)]
        xo = [A("xo", [128, D], F32) for _ in range(2)]
        xTf = [A("xTf", [128, 8, 128], F32) for _ in range(2)]
        junk = A("lnjunk", [128, D], F32)
        st = [A("lst", [128, 8], F32) for _ in range(2)]
        lg = [A("lg", [128, NE], F32) for _ in range(2)]
        top8 = [A("top8", [128, 8], F32) for _ in range(2)]
        ex = [A("ex", [128, NE], F32) for _ in range(2)]
        pm = [P("pm", [128, 512], F32) for _ in range(2)]
        ptr = [P("ptr", [128, 512], F32) for _ in range(2)]
        plg = P("plg", [128, 512], F32)

        S.dma("pool", wod[:], C.w_o[l, 0:512, :].rearrange("(c p) n -> p c n", p=128), [], ["wod"])
        S.dma("sp", wos[:], C.w_o[l, 512:1024, :].rearrange("(h v) n -> v h n", v=64), [], ["wos"])
        S.dma("sp", wuvT[:], C.wuvT[l].rearrange("p (h r) -> p h r", h=8), [], ["wuvT"])
        S.dma("sp", lng[:], C.ln1g[l], [], ["lng"])
        S.dma("sp", lnb[:], C.ln1b[l], [], ["lnb"])
        S.dma("sp", rw[:], C.router_w[l].rearrange("(c p) n -> p c n", p=128), [], ["rw"])
        S.dma("sp", rb[:], C.router_b[l], [], ["rb"])
        for h in range(8):
            for dh in range(2):
                S.mm(pm[dh][:], wuvT[:, h, :], wos[:, h, dh * 512:(dh + 1) * 512], True, True, ["wuvT", "wos"],
                     [("pm", dh)])
                S.cp("act" if dh == 0 else "dve", Wvo[:, h, dh * 512:(dh + 1) * 512], pm[dh][:], [("pm", dh)], ["Wvo"])

        for i in range(NT):
            b = i % 2
            ts_ = slice(i * 128, (i + 1) * 128)
            kxi, kxo = ("xin", b), ("xo", b)
            S.dma("sp", xin[b][:], x_src[ts_, :], [("xsrc", i)], [kxi])
            for dh in range(2):
                ds_ = slice(dh * 512, (dh + 1) * 512)
                for c in range(4):
                    S.mm(pm[dh][:], C.ydT[:, c, ts_], wod[:, c, ds_], c == 0, False, ["ydT", "wod"], [("pm", dh)])
                for h in range(8):
                    S.mm(pm[dh][:], C.olT[:, h, ts_], Wvo[:, h, ds_], False, h == 7, ["olT", "Wvo"], [("pm", dh)])
                S.stt("dve", xin[b][:, ds_], xin[b][:, ds_], ALPHA, pm[dh][:], ALU.mult, ALU.add,
                      [kxi, ("pm", dh)], [kxi])
            kst = ("lst", b)
            layer_norm_tile(C, S, xin[b][:], xo[b][:], lng[:], lnb[:], st[b], kst, [kxi], kxo, junk[:])
            S.dma("sp", x_dst[ts_, :], xo[b][:], [kxo], [("xdst", i)], semkey="xdst")
            kxf = ("xTf", b)
            for hf in range(2):
                for c4 in range(4):
                    dc = hf * 4 + c4
                    S.tr(ptr[hf][:, c4 * 128:(c4 + 1) * 128], xo[b][:, dc * 128:(dc + 1) * 128], C.ident[:], [kxo],
                         [("ptr", hf)])
                S.cp("act", xTf[b][:, hf * 4:(hf + 1) * 4, :], ptr[hf][:].rearrange("p (c t) -> p c t", c=4),
                     [("ptr", hf)], [kxf])
                S.cp("dve", C.xT[:, hf * 4:(hf + 1) * 4, ts_], xTf[b][:, hf * 4:(hf + 1) * 4, :], [kxf], ["xT"])
            for dc in range(8):
                S.mm(plg[:, 0:NE], xTf[b][:, dc, :], rw[:, dc, :], dc == 0, dc == 7, [kxf, "rw"], ["plg"])
            klg = ("lg", b)
            S.tt("dve", lg[b][:], plg[:, 0:NE], rb[:], ALU.add, ["plg", "rb"], [klg])
            S.op("dve", lambda e: e.max(out=top8[b][:], in_=lg[b][:]), [klg], [("top8", b)])
            S.ts("dve", ex[b][:], lg[b][:], top8[b][:, 0:1], None, ALU.subtract, None, [klg, ("top8", b)], [("ex", b)])
            S.act(ex[b][:], ex[b][:], AF.Exp, [("ex", b)], [("ex", b)])
            S.ts("dve", lg[b][:], lg[b][:], top8[b][:, 3:4], None, ALU.is_ge, None, [klg, ("top8", b)], [klg])
            S.tt("dve", ex[b][:], ex[b][:], lg[b][:], ALU.mult, [("ex", b), klg], [("ex", b)])
            S.op("dve", lambda e: e.tensor_reduce(st[b][:, 4:5], ex[b][:], AX.X, ALU.add), [("ex", b)], [kst])
            S.op("dve", lambda e: e.reciprocal(st[b][:, 4:5], st[b][:, 4:5]), [kst], [kst])
            S.ts("dve", C.cw[:, i, :], ex[b][:], st[b][:, 4:5], None, ALU.mult, None, [("ex", b), kst], ["cw"])
            S.tr(plg[0:NE, 128:256], C.cw[:, i, :], C.ident[:], ["cw"], ["plg"])
            S.cp("act", C.cwT[:, ts_], plg[0:NE, 128:256], ["plg"], ["cwT"])
        S.barrier()


def phase_moe(C, l, x_src, x_dst, last):
    nc, S = C.nc, C.S
    xT, cw, cwT = C.xT, C.cw, C.cwT
    with ExitStack() as es:
        A = lambda n, sh, dt: es.enter_context(nc.sbuf_tensor(uid(n), sh, dt))
        P = lambda n, sh, dt: es.enter_context(nc.psum_tensor(uid(n), sh, dt))
        X = A("X", [128, NT, D], F32)
        NB = 2
        Wg = [A("Wg", [128, 8, 256], BF16) for _ in range(NB)]
        Wu = [A("Wu", [128, 8, 256], BF16) for _ in range(NB)]
        Wd = [A("Wd", [128, 2, D], BF16) for _ in range(NB)]
        bgu = A("bgu", [128, NE * 16], F32)
        bdn = A("bdn", [NE, D], F32)
        lng = A("lng", [128, D], F32)
        lnb = A("lnb", [128, D], F32)
        hT = [A("hT", [128, 2, 512], BF16) for _ in range(2)]
        gt = [A("gt", [128, 512], F32) for _ in range(2)]
        sg = [A("sg", [128, 512], F32) for _ in range(2)]
        ut = [A("ut", [128, 512], F32) for _ in range(2)]
        junk = A("lnjunk", [128, D], F32)
        xo = [A("xo", [128, D], F32) for _ in range(2)]
        st = [A("lst", [128, 8], F32) for _ in range(2)]
        pg = [P("pg", [128, 512], F32) for _ in range(2)]
        pu = [P("pu", [128, 512], F32) for _ in range(2)]
        po = [P("po", [128, 512], F32) for _ in range(4)]

        S.dma("sp", bgu[:], C.bgu[l], [], ["bgu"])
        S.dma("sp", bdn[:], C.bdn[l], [], ["bdn"])
        bg2 = A("bg2", [128, NE * 16], F32)
        c119 = A("c119", [128, 1], F32)
        S.op("dve", lambda e: e.memset(c119[:], 1.702 * 7.0), [], ["bg2"])
        bv = bgu[:].rearrange("p (e c) -> p e c", c=16)
        b2v = bg2[:].rearrange("p (e c) -> p e c", c=16)
        S.ts("dve", b2v[:, :, 0:8], bv[:, :, 0:8], -1.0, 7.0, ALU.mult, ALU.add, ["bgu"], ["bg2"])
        S.ts("dve", b2v[:, :, 8:16], bv[:, :, 8:16], 7.0, None, ALU.add, None, ["bgu"], ["bg2"])
        S.dma("sp", lng[:], C.ln2g[l], [], ["lng"])
        S.dma("sp", lnb[:], C.ln2b[l], [], ["lnb"])
        for i in range(NT):
            S.dma("sp", X[:, i, :], x_src[i * 128:(i + 1) * 128, :], [("xdst", i)], [("X", i)])

        steps = [(e, hf) for e in range(NE) for hf in range(4)]

        def load_w(si):
            e, hf = steps[si]
            bi = si % NB
            gv = C.wgu[l, e].rearrange("(c p) n -> p c n", p=128)
            S.dma("pool", Wg[bi][:], gv[:, :, hf * 256:(hf + 1) * 256], [], [("Wg", bi)])
            S.dma("pool", Wu[bi][:], gv[:, :, 1024 + hf * 256:1024 + (hf + 1) * 256], [], [("Wu", bi)])
            S.dma("pool", Wd[bi][:], C.wdn[l, e, hf * 256:(hf + 1) * 256, :].rearrange("(c p) n -> p c n", p=128),
                  [], [("Wd", bi)])

        load_w(0)
        for i in range(NT):
            for dh in range(2):
                ds_ = slice(dh * 512, (dh + 1) * 512)
                S.mm(po[dh][:], cwT[:, i * 128:(i + 1) * 128], bdn[:, ds_], True, True, ["cwT", "bdn"], [("po", dh)])
                S.stt("dve", X[:, i, ds_], X[:, i, ds_], ALPHA, po[dh][:], ALU.mult, ALU.add,
                      [("X", i), ("po", dh)], [("X", i, dh)])

        work = [(si, tg) for si in range(len(steps)) for tg in range(4)]

        acc_t = [A("acct", [128, 512], F32) for _ in range(2)]

        def up_mm(wi, fc):
            si, tg = work[wi]
            bi = si % NB
            tsl = slice(tg * 512, (tg + 1) * 512)
            pb_ = fc
            fs = slice(fc * 128, (fc + 1) * 128)
            for dc in range(8):
                S.mm(pg[pb_][:], Wg[bi][:, dc, fs], xT[:, dc, tsl], dc == 0, dc == 7, [("Wg", bi), "xT"],
                     [("pg", pb_)])
            for dc in range(8):
                S.mm(pu[pb_][:], Wu[bi][:, dc, fs], xT[:, dc, tsl], dc == 0, dc == 7, [("Wu", bi), "xT"],
                     [("pu", pb_)])

        def up_epi(wi, fc):
            si, tg = work[wi]
            e, hf = steps[si]
            hb = wi % 2
            pb_ = fc
            cg = e * 16 + hf * 2 + fc
            cu = e * 16 + 8 + hf * 2 + fc
            kg, ksg, ku = ("gt", pb_), ("sg", pb_), ("ut", pb_)
            S.act(gt[pb_][:], pg[pb_][:], AF.Relu, [("pg", pb_), "bg2"], [kg], scale=-1.0, bias=bg2[:, cg:cg + 1])
            S.act(sg[pb_][:], gt[pb_][:], AF.Sigmoid, [kg, "bg2"], [ksg], scale=-1.702, bias=c119[:, 0:1])
            S.act(ut[pb_][:], pu[pb_][:], AF.Relu, [("pu", pb_), "bg2"], [ku], bias=bg2[:, cu:cu + 1])
            S.stt("dve", gt[pb_][:], gt[pb_][:], 7.0, sg[pb_][:], ALU.subtract, ALU.mult, [kg, ksg], [kg])
            S.ts("dve", ut[pb_][:], ut[pb_][:], 14.0, -6.0, ALU.min, ALU.add, [ku], [ku])
            S.stt("dve", hT[hb][:, fc, :], ut[pb_][:], -1.0, gt[pb_][:], ALU.mult, ALU.mult, [ku, kg], [("hT", hb)])

        def down(wi, tts):
            si, tg = work[wi]
            e, hf = steps[si]
            bi = si % NB
            hb = wi % 2
            for tt in tts:
                i = tg * 4 + tt
                for dh in range(2):
                    ds_ = slice(dh * 512, (dh + 1) * 512)
                    pi = dh * 2 + tt % 2
                    for fc in range(2):
                        S.mm(po[pi][:], hT[hb][:, fc, tt * 128:(tt + 1) * 128], Wd[bi][:, fc, ds_], fc == 0, fc == 1,
                             [("hT", hb), ("Wd", bi)], [("po", pi)])
                    kx = ("X", i, dh)
                    if dh == 0:
                        S.stt("dve", X[:, i, ds_], po[pi][:], cw[:, i, e:e + 1], X[:, i, ds_], ALU.mult, ALU.add,
                              [("po", pi), "cw", kx], [kx])
                    else:
                        ab = tt % 2
                        S.act(acc_t[ab][:], po[pi][:], AF.Copy, [("po", pi), "cw"], [("acct", ab)],
                              scale=cw[:, i, e:e + 1])
                        S.tt("pool", X[:, i, ds_], X[:, i, ds_], acc_t[ab][:], ALU.add, [("acct", ab), kx], [kx])

        nW = len(work)
        for fc in range(2):
            up_mm(0, fc)
            up_epi(0, fc)
        for wi in range(nW):
            si, tg = work[wi]
            if tg == 0 and si + 1 < len(steps):
                load_w(si + 1)
            for half in range(2):
                if wi + 1 < nW:
                    up_mm(wi + 1, half)
                down(wi, [2 * half, 2 * half + 1])
                if wi + 1 < nW:
                    up_epi(wi + 1, half)

        ptm = pg[0][:].bitcast(BF16)
        for i in range(NT):
            b = i % 2
            ts_ = slice(i * 128, (i + 1) * 128)
            kst = ("lst", b)
            kxo = ("xo", b)
            layer_norm_tile(C, S, X[:, i, :], xo[b][:], lng[:], lnb[:], st[b], kst, [("X", i, 0), ("X", i, 1)], kxo, junk[:])
            S.dma("sp", x_dst[ts_, :], xo[b][:], [kxo], [("xsrc", i)], semkey="xout")
            if not last:
                S.cp("pool", C.xb[:], xo[b][:], [kxo], ["xb"])
                for c in range(8):
                    S.tr(ptm[:, c * 128:(c + 1) * 128], C.xb[:, c * 128:(c + 1) * 128], C.identb[:], ["xb"], [("pg", 0)])
                S.cp("act", C.xT[:, :, ts_], ptm[:].rearrange("p (c t) -> p c t", c=8), [("pg", 0)], ["xT"])
        S.barrier()


def phase_in(C, x_src):
    nc, S = C.nc, C.S
    with ExitStack() as es:
        A = lambda n, sh, dt: es.enter_context(nc.sbuf_tensor(uid(n), sh, dt))
        xin = [A("xin", [128, D], BF16) for _ in range(2)]
        pt_ = [es.enter_context(nc.psum_tensor(uid("pti"), [128, 1024], BF16)) for _ in range(2)]
        for i in range(NT):
            b = i % 2
            ts_ = slice(i * 128, (i + 1) * 128)
            S.dma("pool", xin[b][:], x_src[ts_, :], [], [("xin", b)])
            for c in range(8):
                S.tr(pt_[b][:, c * 128:(c + 1) * 128], xin[b][:, c * 128:(c + 1) * 128], C.identb[:], [("xin", b)],
                     [("pti", b)])
            S.cp("act" if b else "dve", C.xT[:, :, ts_], pt_[b][:].rearrange("p (c t) -> p c t", c=8), [("pti", b)],
                 ["xT"])
        S.barrier()


def dbg_dump(C, dbg, x1):
    if not dbg:
        return
    nc, S = C.nc, C.S
    for nm, t in [("d_olT", getattr(C, "olT", None)), ("d_ydT", getattr(C, "ydT", None)), ("d_xT", C.xT)]:
        if t is None:
            continue
        d = nc.dram_tensor(nm, list(t.shape), BF16, kind="ExternalOutput").ap()
        S.dma("sp", d, t[:], [], [nm])
    d = nc.dram_tensor("d_x1", [T, D], F32, kind="ExternalOutput").ap()
    S.dma("sp", d, x1, [], ["d_x1"])
    d = nc.dram_tensor("d_cw", [128, NT, NE], F32, kind="ExternalOutput").ap()
    S.dma("sp", d, C.cw[:], [], ["d_cw"])
    S.barrier()


def build(nl=NL, l0=0, stop=None, dbg=False):
    _, C0 = _build(nl, l0, stop, dbg, None)
    return _build(nl, l0, stop, dbg, C0.S.needed)


def _build(nl, l0, stop, dbg, plan):
    nc = bass.Bass("TRN2", target_bir_lowering=False)
    C = Ctx()
    C.nc = nc

    def din(name, shape):
        return nc.dram_tensor(name, shape, F32, kind="ExternalInput").ap()

    C.x = din("x", [T, D])
    C.w_in = din("w_in", [NL, D, 2512])
    C.convw = din("convw", [NL, 128, 48])
    C.alog = din("alog", [NL, 128, 4])
    C.dtb = din("dtb", [NL, 128, 4])
    C.dnnorm = din("dnnorm", [NL, 128, 128])
    C.qn = din("qn", [NL, 128, 256])
    C.kvn = din("kvn", [NL, 128, 128])
    C.ig = din("ig", [NL, 128, 64])
    C.ib = din("ib", [NL, 128, 64])
    C.wuqT = din("wuqT", [NL, 64, 8 * 256])
    C.wuk = din("wuk", [NL, 64, 8 * 128])
    C.wuvT = din("wuvT", [NL, 64, 8 * 128])
    C.wqidx = din("wqidx", [NL, 256, 512])
    C.w_o = din("w_o", [NL, D, D])
    C.ln1g = din("ln1g", [NL, 128, D])
    C.ln1b = din("ln1b", [NL, 128, D])
    C.ln2g = din("ln2g", [NL, 128, D])
    C.ln2b = din("ln2b", [NL, 128, D])
    C.router_w = din("router_w", [NL, D, NE])
    C.router_b = din("router_b", [NL, 128, NE])
    if stop is None:
        C.wgu = din("wgu", [NL, NE, D, 2048])
        C.bgu = din("bgu", [NL, 128, NE * 16])
        C.wdn = din("wdn", [NL, NE, D, D])
        C.bdn = din("bdn", [NL, NE, D])
    out = nc.dram_tensor("out", [T, D], F32, kind="ExternalOutput").ap()
    xa = nc.dram_tensor("xa_scr", [T, D], F32, kind="Internal").ap()
    x1 = nc.dram_tensor("x1_scr", [T, D], F32, kind="Internal").ap()
    dbg_t = {}
    with ExitStack() as es:
        S = Sched(nc, es, plan)
        C.S = S
        setup_consts(C, es)
        C.xT = es.enter_context(nc.sbuf_tensor("xT", [128, 8, T], BF16))
        C.cw = es.enter_context(nc.sbuf_tensor("cw", [128, NT, NE], F32))
        C.cwT = es.enter_context(nc.sbuf_tensor("cwT", [NE, T], F32))
        C.xb = es.enter_context(nc.sbuf_tensor("xb", [128, D], BF16))
        S.barrier()
        phase_in(C, C.x)
        xsrc = C.x
        for li in range(nl):
            l = l0 + li
            last = li == nl - 1
            with ExitStack() as es2:
                C.olT = es2.enter_context(nc.sbuf_tensor(uid("olT"), [128, 8, T], BF16))
                phase_dsa(C, l)
                if stop == "dsa":
                    dbg_dump(C, dbg, x1)
                    break
                C.ydT = es2.enter_context(nc.sbuf_tensor(uid("ydT"), [128, 4, T], BF16))
                phase_dn(C, l)
                if stop == "dn":
                    dbg_dump(C, dbg, x1)
                    break
                phase_c(C, l, xsrc, x1)
                if stop == "c" or (dbg and last):
                    dbg_dump(C, dbg, x1)
                if stop == "c":
                    break
            xdst = out if last else xa
            phase_moe(C, l, x1, xdst, last)
            xsrc = xa
        S.barrier()
    C.ninst = S.ninst
    C.nwaits = S.nwaits
    return nc, C


def prep_inputs(inp):
    f = lambda a: np.ascontiguousarray(np.asarray(a, dtype=np.float32))
    rep = lambda a: f(np.broadcast_to(np.asarray(a)[:, None, :], (a.shape[0], 128, a.shape[1])))
    sh = {}
    sh["w_in"] = f(inp["w_in"])
    cw = np.asarray(inp["dn_conv"])
    sh["convw"] = f(cw.reshape(NL, 4, 12, 128).transpose(0, 3, 2, 1).reshape(NL, 128, 48))
    sh["alog"] = rep(inp["dn_a_log"])
    sh["dtb"] = rep(inp["dn_dt_bias"])
    sh["dnnorm"] = rep(inp["dn_norm"])
    sh["qn"] = rep(inp["sa_q_norm"])
    sh["kvn"] = rep(inp["sa_kv_norm"])
    sh["ig"] = rep(inp["idx_k_norm_g"])
    sh["ib"] = rep(inp["idx_k_norm_b"])
    wuq = np.asarray(inp["sa_w_uq"])
    sh["wuqT"] = f(wuq.reshape(NL, 256, 8, 64).transpose(0, 3, 2, 1).reshape(NL, 64, 8 * 256))
    wuk = np.asarray(inp["sa_w_uk"])
    sh["wuk"] = f(wuk.transpose(0, 2, 1, 3).reshape(NL, 64, 8 * 128))
    wuv = np.asarray(inp["sa_w_uv"])
    sh["wuvT"] = f(wuv.transpose(0, 3, 1, 2).reshape(NL, 64, 8 * 128))
    sh["wqidx"] = f(inp["idx_w_q"])
    sh["w_o"] = f(inp["w_o"])
    sh["ln1g"] = rep(inp["ln1_g"])
    sh["ln1b"] = rep(inp["ln1_b"])
    sh["ln2g"] = rep(inp["ln2_g"])
    sh["ln2b"] = rep(inp["ln2_b"])
    sh["router_w"] = f(inp["router_w"])
    sh["router_b"] = rep(inp["router_b"])
    sh["wgu"] = f(inp["w_gate_up"])
    bg = np.asarray(inp["b_gate_up"])
    sh["bgu"] = f(bg.reshape(NL, NE, 16, 128).transpose(0, 3, 1, 2).reshape(NL, 128, NE * 16))
    sh["wdn"] = f(inp["w_down"])
    sh["bdn"] = f(inp["b_down"])
    return sh


_cache = {}


def kernel(**inputs):
    n = 8
    x = np.asarray(inputs["x"], dtype=np.float32)
    shared = prep_inputs(inputs)
    if "nc" not in _cache:
        _cache["nc"] = build()[0]
    nc = _cache["nc"]
    in_maps = []
    for c in range(n):
        m = dict(shared)
        m["x"] = np.ascontiguousarray(x[c])
        in_maps.append(m)
    res = run_bass_kernel_spmd(nc, in_maps, core_ids=list(range(n)))
    return np.stack([np.asarray(r["out"], dtype=np.float32) for r in res.results], axis=0)
```
